# Optimizing a Trainium2 kernel written in Bass

```python
import math
import jax, jax.numpy as jnp
from jax import lax
import numpy as np

D_MODEL = 4096
BATCH = 2
SEQ = 4096
DEPTH = 4

CHUNK = 64
Q_BLOCK = 2 * CHUNK
N_HEADS = 32
HEAD_DIM = D_MODEL // N_HEADS
D_FF = 2 * D_MODEL
N_EXPERTS = 8
TOP_K = 2
D_FF_EXPERT = D_MODEL // 2
N_MOD = 6
N_A_LAYERS = DEPTH // 2
N_B_LAYERS = DEPTH - N_A_LAYERS
N_DENSE = (DEPTH + 1) // 2
N_MOE = DEPTH // 2
DEEPNORM_ALPHA = (2.0 * DEPTH) ** 0.25
DEEPNORM_BETA = (8.0 * DEPTH) ** -0.25
LN_EPS = 1e-5
ADA_SCALE = 0.1
FORGET_BIAS = 3.0
NEG_INF = -1e30

kernel_name = "hybrid_stickbreak_fox_yoco_deepnorm_adaln_moe"


def _layernorm(x, g, b):
    xf = x.astype(jnp.float32)
    mu = jnp.mean(xf, axis=-1, keepdims=True)
    var = jnp.mean(jnp.square(xf - mu), axis=-1, keepdims=True)
    y = (xf - mu) * lax.rsqrt(var + LN_EPS)
    return (y * g.astype(jnp.float32) + b.astype(jnp.float32)).astype(x.dtype)


def _modulate(x, shift, scale):
    return x * (1.0 + scale[:, None, :]) + shift[:, None, :]


def _split_heads(t):
    b, s, _ = t.shape
    return t.reshape(b, s, N_HEADS, HEAD_DIM).transpose(0, 2, 1, 3).astype(jnp.float32)


def _merge_heads(t, dtype):
    b, h, s, d = t.shape
    return t.transpose(0, 2, 1, 3).reshape(b, s, h * d).astype(dtype)


def _query_blocks(t):
    b, h, s = t.shape[:3]
    t = t.reshape((b, h, s // Q_BLOCK, Q_BLOCK) + t.shape[3:])
    return jnp.moveaxis(t, 2, 0)


def _unblock(o):
    nb, b, h, qb, d = o.shape
    return jnp.moveaxis(o, 0, 2).reshape(b, h, nb * qb, d)


def _stick_breaking_attention(q, k, v):
    s_len = k.shape[2]
    scale = HEAD_DIM ** -0.5
    key_pos = jnp.arange(s_len)
    nb = s_len // Q_BLOCK

    def block(args):
        qb, start = args
        z = jnp.einsum('bhqd,bhkd->bhqk', qb, k) * scale
        q_pos = start + jnp.arange(Q_BLOCK)
        mask = key_pos[None, :] < q_pos[:, None]
        log_keep = jnp.where(mask, jax.nn.log_sigmoid(-z), 0.0)
        suffix = lax.cumsum(log_keep, axis=3, reverse=True) - log_keep
        w = jnp.where(mask, jnp.exp(jax.nn.log_sigmoid(z) + suffix), 0.0)
        return jnp.einsum('bhqk,bhkd->bhqd', w, v)

    starts = jnp.arange(nb, dtype=jnp.int32) * Q_BLOCK
    out = lax.map(block, (_query_blocks(q), starts))
    return _unblock(out)


def _forgetting_attention(q, k, v, log_f_cum):
    s_len = k.shape[2]
    scale = HEAD_DIM ** -0.5
    key_pos = jnp.arange(s_len)
    nb = s_len // Q_BLOCK

    def block(args):
        qb, fq, start = args
        logits = jnp.einsum('bhqd,bhkd->bhqk', qb, k) * scale
        logits = logits + fq[..., None] - log_f_cum[:, :, None, :]
        q_pos = start + jnp.arange(Q_BLOCK)
        mask = key_pos[None, :] <= q_pos[:, None]
        p = jax.nn.softmax(jnp.where(mask, logits, NEG_INF), axis=-1)
        return jnp.einsum('bhqk,bhkd->bhqd', p, v)

    starts = jnp.arange(nb, dtype=jnp.int32) * Q_BLOCK
    out = lax.map(block, (_query_blocks(q), _query_blocks(log_f_cum), starts))
    return _unblock(out)


def _shared_kv(x, mod, kv_table, kv_w, kv_b_f):
    h = _modulate(x, mod[:, 0] + kv_table[0], mod[:, 1] + kv_table[1])
    kvf = h @ kv_w
    k = _split_heads(kvf[..., :D_MODEL])
    v = _split_heads(kvf[..., D_MODEL:2 * D_MODEL])
    f_logit = (kvf[..., 2 * D_MODEL:] + kv_b_f).astype(jnp.float32)
    log_f_cum = lax.cumsum(jax.nn.log_sigmoid(f_logit), axis=1)
    return k, v, log_f_cum.transpose(0, 2, 1)


def _swiglu(h, w_up, w_down):
    g, u = jnp.split(h @ w_up, 2, axis=-1)
    return (jax.nn.silu(g) * u) @ w_down


def _moe_swiglu(h, w_router, b_router, w_up, w_down):
    b, s, d = h.shape
    t = h.reshape(b * s, d)
    logits = (t @ w_router).astype(jnp.float32) + b_router.astype(jnp.float32)
    top_vals, top_idx = lax.top_k(logits, TOP_K)
    top_w = jax.nn.softmax(top_vals, axis=-1)
    gates = jnp.sum(jax.nn.one_hot(top_idx, N_EXPERTS, dtype=jnp.float32) * top_w[..., None], axis=1)
    gates = gates.astype(h.dtype)
    g, u = jnp.split(jnp.einsum('td,edf->tef', t, w_up), 2, axis=-1)
    act = jax.nn.silu(g) * u * gates[:, :, None]
    y = jnp.einsum('tef,efd->td', act, w_down)
    return y.reshape(b, s, d)


def setup_inputs(seed: int = 0) -> dict:
    key = jax.random.key(seed)
    ks = jax.random.split(key, 23)
    s = D_MODEL ** -0.5

    def nrm(k, shape, std):
        return jax.random.normal(k, shape, jnp.float32) * std

    x = nrm(ks[0], (BATCH, SEQ, D_MODEL), 1.0)
    c = nrm(ks[1], (BATCH, D_MODEL), 1.0)
    ada_w = nrm(ks[2], (D_MODEL, N_MOD * D_MODEL), ADA_SCALE * s)
    ada_b = nrm(ks[3], (N_MOD * D_MODEL,), 0.01)
    ada_table = nrm(ks[4], (DEPTH, N_MOD, D_MODEL), 0.02)
    kv_table = nrm(ks[5], (2, D_MODEL), 0.02)
    a_w_qkv = jnp.concatenate([
        nrm(ks[6], (N_A_LAYERS, D_MODEL, 2 * D_MODEL), s),
        nrm(ks[7], (N_A_LAYERS, D_MODEL, D_MODEL), s * DEEPNORM_BETA)], axis=-1)
    a_w_o = nrm(ks[8], (N_A_LAYERS, D_MODEL, D_MODEL), s * DEEPNORM_BETA)
    kv_w = jnp.concatenate([
        nrm(ks[9], (D_MODEL, D_MODEL), s),
        nrm(ks[10], (D_MODEL, D_MODEL), s * DEEPNORM_BETA),
        nrm(ks[11], (D_MODEL, N_HEADS), s)], axis=-1)
    kv_b_f = FORGET_BIAS + nrm(ks[12], (N_HEADS,), 0.1)
    b_w_q = nrm(ks[13], (N_B_LAYERS, D_MODEL, D_MODEL), s)
    b_w_o = nrm(ks[14], (N_B_LAYERS, D_MODEL, D_MODEL), s * DEEPNORM_BETA)
    ln_g = 1.0 + nrm(ks[15], (DEPTH, 2, D_MODEL), 0.02)
    ln_b = nrm(ks[16], (DEPTH, 2, D_MODEL), 0.02)
    ffn_w_up = nrm(ks[17], (N_DENSE, D_MODEL, 2 * D_FF), s)
    ffn_w_down = nrm(ks[18], (N_DENSE, D_FF, D_MODEL), D_FF ** -0.5 * DEEPNORM_BETA)
    moe_w_router = nrm(ks[19], (N_MOE, D_MODEL, N_EXPERTS), s)
    moe_b_router = nrm(ks[20], (N_MOE, N_EXPERTS), 0.01)
    moe_w_up = nrm(ks[21], (N_MOE, N_EXPERTS, D_MODEL, 2 * D_FF_EXPERT), s)
    moe_w_down = nrm(ks[22], (N_MOE, N_EXPERTS, D_FF_EXPERT, D_MODEL), D_FF_EXPERT ** -0.5 * DEEPNORM_BETA)
    return {"x": x, "c": c, "ada_w": ada_w, "ada_b": ada_b, "ada_table": ada_table,
            "kv_table": kv_table, "a_w_qkv": a_w_qkv, "a_w_o": a_w_o, "kv_w": kv_w,
            "kv_b_f": kv_b_f, "b_w_q": b_w_q, "b_w_o": b_w_o, "ln_g": ln_g, "ln_b": ln_b,
            "ffn_w_up": ffn_w_up, "ffn_w_down": ffn_w_down, "moe_w_router": moe_w_router,
            "moe_b_router": moe_b_router, "moe_w_up": moe_w_up, "moe_w_down": moe_w_down}


def reference(x, c, ada_w, ada_b, ada_table, kv_table, a_w_qkv, a_w_o, kv_w, kv_b_f,
              b_w_q, b_w_o, ln_g, ln_b, ffn_w_up, ffn_w_down, moe_w_router, moe_b_router,
              moe_w_up, moe_w_down):
    bsz = x.shape[0]
    mod = (jax.nn.silu(c) @ ada_w + ada_b).reshape(bsz, N_MOD, D_MODEL)
    shared = None
    for l in range(DEPTH):
        m = mod + ada_table[l][None]
        shift_mix, scale_mix, gate_mix, shift_ffn, scale_ffn, gate_ffn = (m[:, i] for i in range(N_MOD))

        h = _modulate(x, shift_mix, scale_mix)
        if l < N_A_LAYERS:
            q, k, v = jnp.split(h @ a_w_qkv[l], 3, axis=-1)
            o = _stick_breaking_attention(_split_heads(q), _split_heads(k), _split_heads(v))
            mix = _merge_heads(o, x.dtype) @ a_w_o[l]
        else:
            if l == N_A_LAYERS:
                shared = _shared_kv(x, mod, kv_table, kv_w, kv_b_f)
            k_sh, v_sh, log_f_cum = shared
            j = l - N_A_LAYERS
            q = _split_heads(h @ b_w_q[j])
            o = _forgetting_attention(q, k_sh, v_sh, log_f_cum)
            mix = _merge_heads(o, x.dtype) @ b_w_o[j]
        x = _layernorm(DEEPNORM_ALPHA * x + (1.0 + gate_mix[:, None, :]) * mix, ln_g[l, 0], ln_b[l, 0])

        h = _modulate(x, shift_ffn, scale_ffn)
        if l % 2 == 0:
            y = _swiglu(h, ffn_w_up[l // 2], ffn_w_down[l // 2])
        else:
            y = _moe_swiglu(h, moe_w_router[l // 2], moe_b_router[l // 2],
                            moe_w_up[l // 2], moe_w_down[l // 2])
        x = _layernorm(DEEPNORM_ALPHA * x + (1.0 + gate_ffn[:, None, :]) * y, ln_g[l, 1], ln_b[l, 1])
    return x
```

```python
import numpy as np
from contextlib import ExitStack
import concourse.bass as bass
import concourse.mybir as mybir
from concourse.bass_utils import run_bass_kernel_spmd

F32 = mybir.dt.float32
BF16 = mybir.dt.bfloat16
AF = mybir.ActivationFunctionType
ALU = mybir.AluOpType
AX = mybir.AxisListType


class Cfg:
    def __init__(self, D=4096, S=4096, B=2, DEPTH=4, T=512):
        self.D, self.S, self.B, self.DEPTH = D, S, B, DEPTH
        self.H = D // 128
        self.KC = D // 128
        self.T = min(T, S)
        self.NQ = S // self.T
        self.NQB = self.T // 128
        self.NE = 8
        self.DFE = D // 2
        self.GC = self.DFE // 128
        self.NGD = (2 * D) // self.DFE
        self.NA = DEPTH // 2
        self.alpha = (2.0 * DEPTH) ** 0.25
        self.eps = 1e-5
        self.scale = 128 ** -0.5


class Buf:
    __slots__ = ("name", "w", "reads", "dsem", "dcnt")

    def __init__(self, name):
        self.name = name
        self.w = None
        self.reads = {}
        self.dsem = None
        self.dcnt = 0


class Rec:
    def __init__(self):
        self.call = None

    def __getattr__(self, name):
        def f(*a, **k):
            self.call = (name, a, k)
            return self
        return f


def _rec(fn):
    r = Rec()
    fn(r)
    assert r.call is not None
    return r.call


class Sched:
    ENG = ("pe", "act", "dve", "pool", "sp")

    def __init__(self, nc, stack):
        self.nc = nc
        self.stack = stack
        self.prog = {e: [] for e in self.ENG}
        self.esem = {e: stack.enter_context(nc.semaphore("es_" + e)) for e in ("pe", "act", "dve", "pool")}
        self.ecnt = {e: 0 for e in self.esem}
        self.waited = {e: {} for e in self.ENG}
        self.dsems = []
        self.semobj = {}

    def _sid(self, sem):
        k = id(sem)
        self.semobj[k] = sem
        return k

    def _need(self, reads, writes, skip_dma_waw=False):
        need = {}

        def add(tok):
            if tok is None:
                return
            k, v = tok
            if need.get(k, 0) < v:
                need[k] = v
        for b in reads:
            add(b.w)
        for b in writes:
            if not (skip_dma_waw and b.w is not None and b.dsem is not None and b.w[0] == id(b.dsem)):
                add(b.w)
            for k, v in b.reads.items():
                add((k, v))
        return need

    def _wait(self, e, need):
        for k, v in need.items():
            if e == "pe" and k == id(self.esem["pe"]):
                continue
            if self.waited[e].get(k, 0) >= v:
                continue
            self.waited[e][k] = v
            self.prog[e].append(("w", self.semobj[k], v))

    def op(self, e, fn, reads=(), writes=()):
        self._wait(e, self._need(reads, writes))
        self.ecnt[e] += 1
        sem = self.esem[e]
        tok = (self._sid(sem), self.ecnt[e])
        self.prog[e].append(("o", _rec(fn), sem, 1))
        for b in writes:
            b.w = tok
            b.reads = {}
        for b in reads:
            if b.reads.get(tok[0], 0) < tok[1]:
                b.reads[tok[0]] = tok[1]

    def dma(self, q, wbuf, rbuf, fn, waw=False):
        self._wait(q, self._need([rbuf], [wbuf], skip_dma_waw=not waw))
        if wbuf.dsem is None:
            wbuf.dsem = self.stack.enter_context(self.nc.semaphore("ds_%d" % len(self.dsems)))
            self.dsems.append(wbuf)
        wbuf.dcnt += 16
        tok = (self._sid(wbuf.dsem), wbuf.dcnt)
        self.prog[q].append(("o", _rec(fn), wbuf.dsem, 16))
        wbuf.w = tok
        wbuf.reads = {}
        if rbuf.reads.get(tok[0], 0) < tok[1]:
            rbuf.reads[tok[0]] = tok[1]

    def fence(self):
        need = {}
        for e, s in self.esem.items():
            if self.ecnt[e] > 0:
                need[self._sid(s)] = self.ecnt[e]
        for b in self.dsems:
            need[self._sid(b.dsem)] = b.dcnt
        for e in self.ENG:
            self._wait(e, need)

    def emit(self, block):
        nc = self.nc

        def replay(e, eng):
            for it in self.prog[e]:
                if it[0] == "w":
                    eng.wait_ge(it[1], it[2])
                else:
                    name, a, k = it[1]
                    getattr(eng, name)(*a, **k).then_inc(it[2], it[3])

        @block.tensor
        def _(eng):
            replay("pe", eng)

        @block.scalar
        def _(eng):
            replay("act", eng)

        @block.vector
        def _(eng):
            replay("dve", eng)

        @block.gpsimd
        def _(eng):
            replay("pool", eng)

        @block.sync
        def _(eng):
            replay("sp", eng)


def build_nc(cfg):
    c = cfg
    D, S, T, KC, H, NQ, NQB, GC, NE, DEPTH = c.D, c.S, c.T, c.KC, c.H, c.NQ, c.NQB, c.GC, c.NE, c.DEPTH
    NMOE = DEPTH // 2
    NDEN = (DEPTH + 1) // 2
    NB = DEPTH - c.NA
    nc = bass.Bass("TRN2", target_bir_lowering=False)

    def din(name, shape, dt=F32):
        return nc.dram_tensor(name, list(shape), dt, kind="ExternalInput").ap()

    def dint(name, shape, dt):
        return nc.dram_tensor(name, list(shape), dt, kind="Internal").ap()

    xT_in = din("xT", [NQ, KC, 128, T])
    cT_in = din("cT", [128, KC])
    adab_in = din("adabT", [128, 6 * KC])
    tab_in = din("tabT", [128, DEPTH * 6 * KC])
    kvtab_in = din("kvtabT", [128, 2 * KC])
    lng_in = din("lngT", [128, DEPTH * 2 * KC])
    lnb_in = din("lnbT", [128, DEPTH * 2 * KC])
    kvbf_in = din("kvbf", [H, 1])
    rw_in = din("rw", [NMOE, 128, KC, NE])
    rb_in = din("rb", [NMOE, 128, NE])
    cst_in = din("cst", [4, 128, 128])
    w_ada = din("w_ada", [6 * KC, 128, KC, 128])
    w_qkv = din("w_qkv", [c.NA, 3 * KC, 128, KC, 128])
    w_ao = din("w_ao", [c.NA, KC, 128, KC, 128])
    w_kv = din("w_kv", [2 * KC, 128, KC, 128])
    w_kvf = din("w_kvf", [128, KC, H])
    w_bq = din("w_bq", [NB, KC, 128, KC, 128])
    w_bo = din("w_bo", [NB, KC, 128, KC, 128])
    w_fup = din("w_fup", [NDEN, c.NGD * GC * 2, 128, KC, 128])
    w_fdn = din("w_fdn", [NDEN, c.NGD, KC, 128, GC, 128])
    w_mup = din("w_mup", [NMOE, NE * GC * 2, 128, KC, 128])
    w_mdn = din("w_mdn", [NMOE, NE, KC, 128, GC, 128])
    outT = nc.dram_tensor("outT", [NQ, KC, 128, T], F32, kind="ExternalOutput").ap()

    xT_d = dint("xT_d", [NQ, KC, 128, T], F32)
    yT_d = dint("yT_d", [KC, 128, T], F32)
    acc_d = dint("acc_d", [KC, 128, T], F32)
    qT_d = dint("qT_d", [H, 128, T], BF16)
    kT_d = dint("kT_d", [H, 128, S], BF16)
    v_d = dint("v_d", [H, S, 128], BF16)
    F_d = dint("F_d", [H, S], F32)

    with ExitStack() as st:
        def sb(name, shape, dt):
            return st.enter_context(nc.sbuf_tensor("sb_" + name, list(shape), dt))

        def ps(name):
            return st.enter_context(nc.psum_tensor(name, [128, 512], F32))

        sc = Sched(nc, st)
        SMAX = max(S, KC * 128)
        AT = sb("AT", [128, KC, T], BF16)
        S0 = sb("S0", [128, SMAX], F32)
        S1 = sb("S1", [128, SMAX], F32)
        S2 = sb("S2", [128, max(SMAX, NE * T)], F32)
        R1 = sb("R1", [128, max(2 * S, GC * T)], BF16)
        R2 = sb("R2", [128, max(2 * S, 2 * KC * 128)], BF16)
        QQ = [sb("QQ%d" % i, [128, T], BF16) for i in range(2)]
        Wt = sb("Wt", [128, S], BF16)
        WT = [sb("WT%d" % i, [128, 512], BF16) for i in range(2)]
        FTq = sb("FTq", [H, T], F32)
        Flast = sb("Flast", [H, 1], F32)
        Ftok = sb("Ftok", [128, S // 128, H], F32)
        cst = sb("cst", [128, 4, 128], F32)
        cstb = sb("cstb", [128, 4, 128], BF16)
        small = {n: sb(n, [128, w], F32) for n, w in (
            ("cT", KC), ("adab", 6 * KC), ("tab", DEPTH * 6 * KC), ("kvtab", 2 * KC), ("lng", DEPTH * 2 * KC),
            ("lnb", DEPTH * 2 * KC), ("modT", 6 * KC), ("LS", DEPTH * 6 * KC), ("KVS", 2 * KC), ("AB", 2 * KC))}
        scb = sb("scb", [128, KC], BF16)
        kvbf = sb("kvbf", [H, 1], F32)
        RW = sb("RW", [128, KC, NE], F32)
        RB = sb("RB", [128, NE], F32)
        WFf = sb("WFf", [128, KC, H], F32)
        WFb = sb("WFb", [128, KC, H], BF16)
        XS = [sb("XS%d" % i, [128, T], F32) for i in range(2)]
        XA = sb("XA", [128, T], F32)
        YS = [sb("YS%d" % i, [128, T], F32) for i in range(2)]
        YB = sb("YB", [128, T], BF16)
        YQ = sb("YQ", [128, T], BF16)
        MEAN = sb("MEAN", [128, T], F32)
        RSTD = sb("RSTD", [128, T], F32)
        NMR = sb("NMR", [128, T], F32)
        TN = sb("TN", [128, T], F32)
        HF = sb("HF", [128, T], F32)
        SG = sb("SG", [128, T], F32)
        QS = [sb("QS%d" % i, [128, T], BF16) for i in range(2)]
        VTt = [sb("VTt%d" % i, [128, NQB, 128], BF16) for i in range(2)]
        FE = sb("FE", [H, T], F32)
        sm = {n: sb(n, [128, w], F32) for n, w in (("npt", 1), ("rs", 1), ("rinv", 1), ("lg", 8), ("m8", 8),
                                                    ("sel", 8), ("ex", 8), ("nm1", 1), ("G", 8))}
        DGf = sb("DGf", [128, 128], F32)
        DGb = sb("DGb", [128, 128], BF16)
        PSL = [ps("psL0"), ps("psL1")]
        PSS, PSQ, PSR, PSG, PSO = ps("psS"), ps("psQ"), ps("psR"), ps("psG"), ps("psO")
        PST = [PSS, PSQ]

        B = {}

        def bf(name):
            if name not in B:
                B[name] = Buf(name)
            return B[name]

        SPq, PLq = "sp", "pool"
        IDf, IDb = cst[:, 0, :], cstb[:, 0, :]
        MLTf, MLTb, MLEb = cst[:, 1, :], cstb[:, 1, :], cstb[:, 2, :]
        ONf, ONb = cst[:, 3, :], cstb[:, 3, :]

        def load(dst_ap, dst_b, src_ap, src_b, q=SPq):
            sc.dma(q, dst_b, src_b, lambda e, o=dst_ap, i=src_ap: e.dma_start(out=o, in_=i))

        load(cst[:], bf("cst"), cst_in.rearrange("c p n -> p c n"), bf("cst_in"))
        sc.op("dve", lambda e: e.tensor_copy(out=cstb[:], in_=cst[:]), [bf("cst")], [bf("cstb")])
        for n, src in (("cT", cT_in), ("adab", adab_in), ("tab", tab_in), ("kvtab", kvtab_in), ("lng", lng_in),
                       ("lnb", lnb_in)):
            load(small[n][:], bf(n), src, bf(n + "_in"))
        load(kvbf[:], bf("kvbf"), kvbf_in, bf("kvbf_in"))
        load(WFf[:], bf("WFf"), w_kvf, bf("w_kvf"))
        sc.op("dve", lambda e: e.tensor_copy(out=WFb[:], in_=WFf[:]), [bf("WFf")], [bf("WFb")])
        sc.op("dve", lambda e: e.tensor_scalar(out=kvbf[:], in0=kvbf[:], scalar1=-1.0, scalar2=None, op0=ALU.mult),
              [bf("kvbf")], [bf("kvbf")])
        sc.op("dve", lambda e: e.memset(S2[:, 0:1], 0.0), [], [bf("S2")])
        sc.op("act", lambda e: e.activation(out=scb[:], in_=small["cT"][:], func=AF.Silu), [bf("cT")], [bf("scb")])

        WFs = [S0, S1]
        WFb_ = [bf("S0"), bf("S1")]
        WBs = [R2[:, 0:KC * 128], R2[:, KC * 128:2 * KC * 128]]
        WBb_ = [bf("R2a"), bf("R2b")]
        PSLb = [bf("psL0"), bf("psL1")]

        def linear(tile_fn, nslabs, kcin, rhs_fn, N, epilogue, wbuf):
            pending = None

            def ld(s):
                i = s % 2
                load(WFs[i][:, 0:kcin * 128], WFb_[i], tile_fn(s).rearrange("p k n -> p (k n)"), wbuf)
            ld(0)
            for s in range(nslabs):
                i = s % 2
                if s + 1 < nslabs:
                    ld(s + 1)
                sc.op("pool", lambda e, i=i: e.tensor_copy(out=WBs[i][:, 0:kcin * 128], in_=WFs[i][:, 0:kcin * 128]),
                      [WFb_[i]], [WBb_[i]])
                if pending is not None:
                    pending()
                    pending = None
                for kc in range(kcin):
                    rap, rb = rhs_fn(kc)
                    sc.op("pe", lambda e, i=i, kc=kc, rap=rap: e.matmul(
                        PSL[i][:, 0:N], lhsT=WBs[i][:, kc * 128:(kc + 1) * 128], rhs=rap,
                        start=(kc == 0), stop=(kc == kcin - 1)), [WBb_[i], rb], [PSLb[i]])
                pending = epilogue(s, PSL[i][:, 0:N], PSLb[i])
            if pending is not None:
                pending()
            sc.fence()

        def ep_mod(s, pa, pb):
            sc.op("dve", lambda e: e.tensor_tensor(out=small["modT"][:, s:s + 1], in0=pa, in1=small["adab"][:, s:s + 1],
                                                   op=ALU.add), [pb, bf("adab")], [bf("modT")])
            return None
        linear(lambda s: w_ada[s], 6 * KC, KC, lambda kc: (scb[:, kc:kc + 1], bf("scb")), 1, ep_mod, bf("w_ada"))
        LS = small["LS"]
        for l in range(DEPTH):
            o = l * 6 * KC
            sc.op("dve", lambda e, o=o: e.tensor_tensor(out=LS[:, o:o + 6 * KC], in0=small["modT"][:],
                                                        in1=small["tab"][:, o:o + 6 * KC], op=ALU.add),
                  [bf("modT"), bf("tab")], [bf("LS")])
            for i in (1, 2, 4, 5):
                sc.op("dve", lambda e, a=o + i * KC: e.tensor_scalar(out=LS[:, a:a + KC], in0=LS[:, a:a + KC], scalar1=1.0,
                                                                   scalar2=None, op0=ALU.add), [bf("LS")], [bf("LS")])
        KVS = small["KVS"]
        sc.op("dve", lambda e: e.tensor_tensor(out=KVS[:], in0=small["modT"][:, 0:2 * KC], in1=small["kvtab"][:],
                                               op=ALU.add), [bf("modT"), bf("kvtab")], [bf("KVS")])
        sc.op("dve", lambda e: e.tensor_scalar(out=KVS[:, KC:2 * KC], in0=KVS[:, KC:2 * KC], scalar1=1.0, scalar2=None,
                                               op0=ALU.add), [bf("KVS")], [bf("KVS")])
        sc.fence()

        def lsc(l, i, s):
            a = (l * 6 + i) * KC + s
            return LS[:, a:a + 1]

        ATb = [bf("AT%d" % k) for k in range(KC)]

        def x_src(l, q):
            return (xT_in, bf("xT_in")) if l == 0 else (xT_d, bf("xT_d%d" % q))

        def modpass(src, srcb, q, shift_fn, sc1_fn, sbufs):
            for k in range(KC):
                i = k % 2
                load(XS[i][:], bf("XS%d" % i), src[q, k], srcb)
                sc.op("act", lambda e, i=i, k=k: e.activation(out=AT[:, k, :], in_=XS[i][:], func=AF.Identity,
                                                              bias=shift_fn(k), scale=sc1_fn(k)),
                      [bf("XS%d" % i)] + sbufs, [ATb[k]])
            sc.fence()

        def at_rhs(kc):
            return AT[:, kc, :], ATb[kc]

        def store_fm(s_tile, s_buf, dst_ap, dst_b):
            def f():
                load(dst_ap, dst_b, s_tile, s_buf, q=PLq)
            return f

        def ep_q(s, pa, pb):
            i = s % 2
            sc.op("act", lambda e: e.activation(out=QS[i][:], in_=pa, func=AF.Copy), [pb], [bf("QS%d" % i)])
            return store_fm(QS[i][:], bf("QS%d" % i), qT_d[s % KC], bf("qT_d"))

        def ep_k(q):
            def f(s, pa, pb):
                i = s % 2
                sc.op("act", lambda e: e.activation(out=QS[i][:], in_=pa, func=AF.Copy), [pb], [bf("QS%d" % i)])
                return store_fm(QS[i][:], bf("QS%d" % i), kT_d[s % KC][:, q * T:(q + 1) * T], bf("kT_d"))
            return f

        def ep_v(q):
            def f(s, pa, pb):
                i = s % 2
                sc.op("act", lambda e: e.activation(out=QS[i][:], in_=pa, func=AF.Copy), [pb], [bf("QS%d" % i)])
                for tb in range(NQB):
                    sc.op("pe", lambda e, tb=tb: e.matmul(PSG[:, tb * 128:(tb + 1) * 128],
                                                          lhsT=QS[i][:, tb * 128:(tb + 1) * 128], rhs=IDb,
                                                          start=True, stop=True), [bf("QS%d" % i), bf("cstb")], [bf("psG")])
                sc.op("dve", lambda e: e.tensor_copy(out=VTt[i][:].rearrange("p a d -> p (a d)"), in_=PSG[:, 0:T]),
                      [bf("psG")], [bf("VTt%d" % i)])
                dst = v_d[s % KC][q * T:(q + 1) * T, :].rearrange("(a p) d -> p a d", p=128)
                return store_fm(VTt[i][:], bf("VTt%d" % i), dst, bf("v_d"))
            return f

        def ep_qkv(q):
            fk, fv = ep_k(q), ep_v(q)

            def f(s, pa, pb):
                return (ep_q, fk, fv)[s // KC](s, pa, pb)
            return f

        def ep_kv(q):
            fk, fv = ep_k(q), ep_v(q)

            def f(s, pa, pb):
                return (fk, fv)[s // KC](s, pa, pb)
            return f

        KTs = [R1[:, 0:S], R1[:, S:2 * S]]
        VVs = [R2[:, 0:S], R2[:, S:2 * S]]
        scale = c.scale

        def attention(q, fox):
            Lq = (q + 1) * T
            nkb_q = Lq // 128
            for h in range(H):
                i = h % 2
                KTb, VVb, QQb = bf("KT%d" % i), bf("VV%d" % i), bf("QQ%d" % i)
                load(KTs[i][:, 0:Lq], KTb, kT_d[h][:, 0:Lq], bf("kT_d"))
                load(VVs[i][:, 0:Lq].rearrange("p (a d) -> p a d", d=128), VVb,
                     v_d[h][0:Lq, :].rearrange("(a p) d -> p a d", p=128), bf("v_d"))
                load(QQ[i][:], QQb, qT_d[h], bf("qT_d"))
                if fox:
                    NFRi, NFRb = (S0, S1)[i], bf("S%d" % i)
                    load(NFRi[0:1, 0:Lq], NFRb, F_d[h:h + 1, 0:Lq], bf("F_d"))
                    sc.op("dve", lambda e: e.tensor_scalar(out=NFRi[0:1, 0:Lq], in0=NFRi[0:1, 0:Lq], scalar1=-1.0 / scale,
                                                           scalar2=None, op0=ALU.mult), [NFRb], [NFRb])
                else:
                    sc.op("dve", lambda e: e.memset(S2[:, 0:1], 0.0), [], [bf("S2")])
                for qb in range(NQB):
                    g = q * NQB + qb
                    L = (g + 1) * 128
                    chunks = [(c0, min(512, L - c0)) for c0 in range(0, L, 512)]
                    qap = QQ[i][:, qb * 128:(qb + 1) * 128]

                    def zmm(ci, c0, w, with_f):
                        pi = ci % 2
                        sc.op("pe", lambda e: e.matmul(PSL[pi][:, 0:w], lhsT=qap, rhs=KTs[i][:, c0:c0 + w], start=True,
                                                       stop=not with_f), [QQb, KTb], [PSLb[pi]])
                        if with_f:
                            sc.op("pe", lambda e: e.matmul(PSL[pi][:, 0:w], lhsT=ONf[0:1, :], rhs=NFRi[0:1, c0:c0 + w],
                                                           start=False, stop=True), [bf("cst"), NFRb], [PSLb[pi]])
                        return pi
                    if not fox:
                        for ci, (c0, w) in enumerate(chunks):
                            pi = zmm(ci, c0, w, False)
                            sc.op("act", lambda e, pi=pi, c0=c0, w=w: e.activation(out=S0[:, c0:c0 + w], in_=PSL[pi][:, 0:w],
                                                                                func=AF.Exp, scale=scale),
                                  [PSLb[pi]], [bf("S0")])
                            sc.op("act", lambda e, c0=c0, w=w: e.activation(out=S1[:, c0:c0 + w], in_=S0[:, c0:c0 + w],
                                                                            func=AF.Ln, bias=1.0), [bf("S0")], [bf("S1")])
                        sc.op("pool", lambda e: e.tensor_tensor(out=S1[:, L - 128:L], in0=S1[:, L - 128:L], in1=MLTf,
                                                                op=ALU.mult), [bf("S1"), bf("cst")], [bf("S1")])
                        if L > 1:
                            sc.op("dve", lambda e: e.tensor_tensor_scan(out=S2[:, 1:L], data0=S1[:, 0:L - 1],
                                                                        data1=S1[:, 0:L - 1], initial=0.0, op0=ALU.add,
                                                                        op1=ALU.max), [bf("S1")], [bf("S2")])
                        sc.op("dve", lambda e: e.scalar_tensor_tensor(out=sm["npt"][:], in0=S2[:, L - 1:L], scalar=-1.0,
                                                                      in1=S1[:, L - 1:L], op0=ALU.mult, op1=ALU.subtract),
                              [bf("S1"), bf("S2")], [bf("npt")])
                        for ci, (c0, w) in enumerate(chunks):
                            pi = zmm(ci, c0, w, False)
                            sc.op("dve", lambda e, pi=pi, c0=c0, w=w: e.scalar_tensor_tensor(
                                out=S0[:, c0:c0 + w], in0=PSL[pi][:, 0:w], scalar=scale, in1=S2[:, c0:c0 + w],
                                op0=ALU.mult, op1=ALU.add), [PSLb[pi], bf("S2")], [bf("S0")])
                            sc.op("act", lambda e, c0=c0, w=w: e.activation(out=Wt[:, c0:c0 + w], in_=S0[:, c0:c0 + w],
                                                                            func=AF.Exp, bias=sm["npt"][:]),
                                  [bf("S0"), bf("npt")], [bf("Wt")])
                        sc.op("pool", lambda e: e.tensor_tensor(out=Wt[:, L - 128:L], in0=Wt[:, L - 128:L], in1=MLTb,
                                                                op=ALU.mult), [bf("Wt"), bf("cstb")], [bf("Wt")])
                        tr_rhs, tr_b = IDb, bf("cstb")
                    else:
                        for ci, (c0, w) in enumerate(chunks):
                            pi = zmm(ci, c0, w, True)
                            sc.op("act", lambda e, pi=pi, c0=c0, w=w: e.activation(
                                out=Wt[:, c0:c0 + w], in_=PSL[pi][:, 0:w], func=AF.Exp, scale=scale,
                                bias=Ftok[:, g, h:h + 1]), [PSLb[pi], bf("Ftok")], [bf("Wt")])
                        sc.op("pool", lambda e: e.tensor_tensor(out=Wt[:, L - 128:L], in0=Wt[:, L - 128:L], in1=MLEb,
                                                                op=ALU.mult), [bf("Wt"), bf("cstb")], [bf("Wt")])
                        sc.op("dve", lambda e: e.reduce_sum(out=sm["rs"][:], in_=Wt[:, 0:L], axis=AX.X), [bf("Wt")], [bf("rs")])
                        sc.op("dve", lambda e: e.reciprocal(out=sm["rinv"][:], in_=sm["rs"][:]), [bf("rs")], [bf("rinv")])
                        sc.op("dve", lambda e: e.tensor_scalar(out=DGb[:], in0=IDf, scalar1=sm["rinv"][:], scalar2=None,
                                                               op0=ALU.mult), [bf("cst"), bf("rinv")], [bf("DGb")])
                        tr_rhs, tr_b = DGb[:], bf("DGb")
                    for b0 in range(0, g + 1, 4):
                        nb = min(4, g + 1 - b0)
                        ti = (b0 // 4) % 2
                        tb_ = bf("psS") if ti == 0 else bf("psQ")
                        for j in range(nb):
                            kb = b0 + j
                            sc.op("pe", lambda e, j=j, kb=kb: e.matmul(PST[ti][:, j * 128:(j + 1) * 128],
                                                                       lhsT=Wt[:, kb * 128:(kb + 1) * 128], rhs=tr_rhs,
                                                                       start=True, stop=True), [bf("Wt"), tr_b], [tb_])
                        sc.op("act", lambda e, nb=nb: e.activation(out=WT[ti][:, 0:nb * 128], in_=PST[ti][:, 0:nb * 128],
                                                                   func=AF.Copy), [tb_], [bf("WT%d" % ti)])
                        for j in range(nb):
                            kb = b0 + j
                            sc.op("pe", lambda e, j=j, kb=kb: e.matmul(PSO[:, 0:128], lhsT=VVs[i][:, kb * 128:(kb + 1) * 128],
                                                                       rhs=WT[ti][:, j * 128:(j + 1) * 128], start=(kb == 0),
                                                                       stop=(kb == g)), [VVb, bf("WT%d" % ti)], [bf("psO")])
                    sc.op("dve", lambda e: e.tensor_copy(out=AT[:, h, qb * 128:(qb + 1) * 128], in_=PSO[:, 0:128]),
                          [bf("psO")], [ATb[h]])
            sc.fence()

        def res_stats(s, src_ap, src_b, gate_ap, xsrc, xsrcb, q):
            i = s % 2
            load(XS[i][:], bf("XS%d" % i), xsrc[q, s], xsrcb)
            sc.op("act", lambda e: e.activation(out=XA[:], in_=XS[i][:], func=AF.Identity, scale=c.alpha), [bf("XS%d" % i)],
                  [bf("XA")])
            sc.op("dve", lambda e: e.scalar_tensor_tensor(out=YS[i][:], in0=src_ap, scalar=gate_ap, in1=XA[:],
                                                          op0=ALU.mult, op1=ALU.add), [src_b, bf("XA"), bf("LS")],
                  [bf("YS%d" % i)])
            sc.op("act", lambda e: e.activation(out=YB[:], in_=YS[i][:], func=AF.Copy), [bf("YS%d" % i)], [bf("YB")])
            sc.op("act", lambda e: e.activation(out=YQ[:], in_=YS[i][:], func=AF.Square), [bf("YS%d" % i)], [bf("YQ")])
            sc.op("pe", lambda e: e.matmul(PSS[:, 0:T], lhsT=ONb, rhs=YB[:], start=(s == 0), stop=(s == KC - 1)),
                  [bf("cstb"), bf("YB")], [bf("psS")])
            sc.op("pe", lambda e: e.matmul(PSQ[:, 0:T], lhsT=ONb, rhs=YQ[:], start=(s == 0), stop=(s == KC - 1)),
                  [bf("cstb"), bf("YQ")], [bf("psQ")])
            return store_fm(YS[i][:], bf("YS%d" % i), yT_d[s], bf("yT_d"))

        def ln_finish():
            sc.op("dve", lambda e: e.tensor_scalar(out=MEAN[:], in0=PSS[:, 0:T], scalar1=1.0 / D, scalar2=None,
                                                   op0=ALU.mult), [bf("psS")], [bf("MEAN")])
            sc.op("dve", lambda e: e.tensor_tensor(out=TN[:], in0=MEAN[:], in1=MEAN[:], op=ALU.mult), [bf("MEAN")], [bf("TN")])
            sc.op("dve", lambda e: e.scalar_tensor_tensor(out=RSTD[:], in0=PSQ[:, 0:T], scalar=1.0 / D, in1=TN[:],
                                                          op0=ALU.mult, op1=ALU.subtract), [bf("psQ"), bf("TN")], [bf("RSTD")])
            sc.op("dve", lambda e: e.tensor_scalar(out=RSTD[:], in0=RSTD[:], scalar1=c.eps, scalar2=None, op0=ALU.add),
                  [bf("RSTD")], [bf("RSTD")])
            sc.op("act", lambda e: e.activation(out=RSTD[:], in_=RSTD[:], func=AF.Sqrt), [bf("RSTD")], [bf("RSTD")])
            sc.op("dve", lambda e: e.reciprocal(out=RSTD[:], in_=RSTD[:]), [bf("RSTD")], [bf("RSTD")])
            sc.op("dve", lambda e: e.scalar_tensor_tensor(out=NMR[:], in0=MEAN[:], scalar=-1.0, in1=RSTD[:], op0=ALU.mult,
                                                          op1=ALU.mult), [bf("MEAN"), bf("RSTD")], [bf("NMR")])
            sc.fence()

        def ln_apply(l, j, q, dst, dstb, make_h, router):
            AB = small["AB"]
            if make_h:
                go = (l * 2 + j) * KC
                sc.op("dve", lambda e: e.tensor_tensor(out=AB[:, 0:KC], in0=small["lng"][:, go:go + KC],
                                                       in1=LS[:, (l * 6 + 4) * KC:(l * 6 + 5) * KC], op=ALU.mult),
                      [bf("lng"), bf("LS")], [bf("AB")])
                sc.op("dve", lambda e: e.tensor_tensor(out=AB[:, KC:2 * KC], in0=small["lnb"][:, go:go + KC],
                                                       in1=LS[:, (l * 6 + 4) * KC:(l * 6 + 5) * KC], op=ALU.mult),
                      [bf("lnb"), bf("LS")], [bf("AB")])
                sc.op("dve", lambda e: e.tensor_tensor(out=AB[:, KC:2 * KC], in0=AB[:, KC:2 * KC],
                                                       in1=LS[:, (l * 6 + 3) * KC:(l * 6 + 4) * KC], op=ALU.add),
                      [bf("AB"), bf("LS")], [bf("AB")])
            pend = None
            for s in range(KC):
                i = s % 2
                go = (l * 2 + j) * KC + s
                load(YS[i][:], bf("YS%d" % i), yT_d[s], bf("yT_d"))
                sc.op("dve", lambda e: e.tensor_tensor(out=TN[:], in0=YS[i][:], in1=RSTD[:], op=ALU.mult),
                      [bf("YS%d" % i), bf("RSTD")], [bf("TN")])
                sc.op("dve", lambda e: e.tensor_tensor(out=TN[:], in0=TN[:], in1=NMR[:], op=ALU.add), [bf("TN"), bf("NMR")],
                      [bf("TN")])
                sc.op("dve", lambda e: e.tensor_scalar(out=XS[i][:], in0=TN[:], scalar1=small["lng"][:, go:go + 1],
                                                       scalar2=small["lnb"][:, go:go + 1], op0=ALU.mult, op1=ALU.add),
                      [bf("TN"), bf("lng"), bf("lnb")], [bf("XS%d" % i)])
                if pend is not None:
                    pend()
                pend = store_fm(XS[i][:], bf("XS%d" % i), dst[q, s], dstb)
                if make_h:
                    sc.op("dve", lambda e: e.tensor_scalar(out=HF[:], in0=TN[:], scalar1=AB[:, s:s + 1],
                                                           scalar2=AB[:, KC + s:KC + s + 1], op0=ALU.mult, op1=ALU.add),
                          [bf("TN"), bf("AB")], [bf("HF")])
                    sc.op("act", lambda e: e.activation(out=AT[:, s, :], in_=HF[:], func=AF.Copy), [bf("HF")], [ATb[s]])
                    if router:
                        for tb in range(NQB):
                            sc.op("pe", lambda e, tb=tb: e.matmul(
                                PSR[:, tb * NE:(tb + 1) * NE], lhsT=HF[:, tb * 128:(tb + 1) * 128], rhs=RW[:, s, :],
                                start=(s == 0 and tb == 0), stop=(s == KC - 1 and tb == NQB - 1), skip_group_check=True),
                                [bf("HF"), bf("RW")], [bf("psR")])
            pend()
            sc.fence()

        def gates():
            for tb in range(NQB):
                sc.op("dve", lambda e: e.tensor_tensor(out=sm["lg"][:], in0=PSR[:, tb * NE:(tb + 1) * NE], in1=RB[:],
                                                       op=ALU.add), [bf("psR"), bf("RB")], [bf("lg")])
                sc.op("dve", lambda e: e.max(out=sm["m8"][:], in_=sm["lg"][:]), [bf("lg")], [bf("m8")])
                sc.op("dve", lambda e: e.tensor_scalar(out=sm["sel"][:], in0=sm["lg"][:], scalar1=sm["m8"][:, 1:2],
                                                       scalar2=None, op0=ALU.is_ge), [bf("lg"), bf("m8")], [bf("sel")])
                sc.op("dve", lambda e: e.tensor_scalar(out=sm["nm1"][:], in0=sm["m8"][:, 0:1], scalar1=-1.0, scalar2=None,
                                                       op0=ALU.mult), [bf("m8")], [bf("nm1")])
                sc.op("act", lambda e: e.activation(out=sm["ex"][:], in_=sm["lg"][:], func=AF.Exp, bias=sm["nm1"][:]),
                      [bf("lg"), bf("nm1")], [bf("ex")])
                sc.op("dve", lambda e: e.tensor_tensor(out=sm["ex"][:], in0=sm["ex"][:], in1=sm["sel"][:], op=ALU.mult),
                      [bf("ex"), bf("sel")], [bf("ex")])
                sc.op("dve", lambda e: e.reduce_sum(out=sm["rs"][:], in_=sm["ex"][:], axis=AX.X), [bf("ex")], [bf("rs")])
                sc.op("dve", lambda e: e.reciprocal(out=sm["rinv"][:], in_=sm["rs"][:]), [bf("rs")], [bf("rinv")])
                sc.op("dve", lambda e: e.tensor_scalar(out=sm["G"][:], in0=sm["ex"][:], scalar1=sm["rinv"][:], scalar2=None,
                                                       op0=ALU.mult), [bf("ex"), bf("rinv")], [bf("G")])
                for ex in range(NE):
                    sc.op("dve", lambda e, ex=ex: e.tensor_scalar(out=DGf[:], in0=IDf, scalar1=sm["G"][:, ex:ex + 1],
                                                                  scalar2=None, op0=ALU.mult), [bf("cst"), bf("G")], [bf("DGf")])
                    sc.op("pe", lambda e: e.matmul(PSG[:, 0:128], lhsT=ONf, rhs=DGf[:], start=True, stop=True),
                          [bf("cst"), bf("DGf")], [bf("psG")])
                    sc.op("dve", lambda e, ex=ex: e.tensor_copy(out=S2[:, ex * T + tb * 128:ex * T + (tb + 1) * 128],
                                                                in_=PSG[:, 0:128]), [bf("psG")], [bf("S2")])
            sc.fence()

        ACTb = [bf("ACT%d" % k) for k in range(GC)]

        def ffn(l, q, moe):
            ng = NE if moe else c.NGD
            wi = l // 2
            wup = w_mup[wi] if moe else w_fup[wi]
            wdn = w_mdn[wi] if moe else w_fdn[wi]
            for g in range(ng):
                def ep_up(s, pa, pb):
                    jj = s // 2
                    if s % 2 == 0:
                        sc.op("act", lambda e: e.activation(out=SG[:], in_=pa, func=AF.Silu), [pb], [bf("SG")])
                    else:
                        sc.op("dve", lambda e: e.tensor_tensor(out=R1[:, jj * T:(jj + 1) * T], in0=SG[:], in1=pa, op=ALU.mult),
                              [bf("SG"), pb], [ACTb[jj]])
                    return None
                linear(lambda s, g=g: wup[g * GC * 2 + s], GC * 2, KC, at_rhs, T, ep_up, bf("w_up"))

                def ep_dn(s, pa, pb, g=g):
                    i = s % 2
                    if moe:
                        sc.op("dve", lambda e: e.tensor_tensor(out=YS[i][:], in0=pa, in1=S2[:, g * T:(g + 1) * T], op=ALU.mult),
                              [pb, bf("S2")], [bf("YS%d" % i)])
                    else:
                        sc.op("dve", lambda e: e.tensor_copy(out=YS[i][:], in_=pa), [pb], [bf("YS%d" % i)])

                    def f():
                        if g == 0:
                            load(acc_d[s], bf("acc_d"), YS[i][:], bf("YS%d" % i), q=PLq)
                        else:
                            sc.dma(PLq, bf("acc_d"), bf("YS%d" % i),
                                   lambda e: e.dma_start(out=acc_d[s], in_=YS[i][:], accum_op=ALU.add))
                    return f
                linear(lambda s, g=g: wdn[g, s], KC, GC, lambda kc: (R1[:, kc * T:(kc + 1) * T], ACTb[kc]), T, ep_dn,
                       bf("w_dn"))

        ri = 0
        for l in range(DEPTH):
            sb_layer = l < c.NA
            moe = (l % 2 == 1)
            last = (l == DEPTH - 1)
            if moe:
                load(RW[:], bf("RW"), rw_in[l // 2], bf("rw_in"))
                load(RB[:], bf("RB"), rb_in[l // 2], bf("rb_in"))
            if l == c.NA:
                for q in range(NQ):
                    src, srcb = x_src(l, q)
                    modpass(src, srcb, q, lambda k: KVS[:, k:k + 1], lambda k: KVS[:, KC + k:KC + k + 1], [bf("KVS")])
                    linear(lambda s: w_kv[s], 2 * KC, KC, at_rhs, T, ep_kv(q), bf("w_kv"))
                    for kc in range(KC):
                        sc.op("pe", lambda e, kc=kc: e.matmul(PSG[0:H, 0:T], lhsT=WFb[:, kc, :], rhs=AT[:, kc, :],
                                                              start=(kc == 0), stop=(kc == KC - 1)), [bf("WFb"), ATb[kc]],
                              [bf("psG")])
                    sc.op("act", lambda e: e.activation(out=FE[:], in_=PSG[0:H, 0:T], func=AF.Exp, scale=-1.0, bias=kvbf[:]),
                          [bf("psG"), bf("kvbf")], [bf("FE")])
                    sc.op("act", lambda e: e.activation(out=FE[:], in_=FE[:], func=AF.Ln, bias=1.0), [bf("FE")], [bf("FE")])
                    sc.op("dve", lambda e: e.tensor_scalar(out=FE[:], in0=FE[:], scalar1=-1.0, scalar2=None, op0=ALU.mult),
                          [bf("FE")], [bf("FE")])
                    sc.op("dve", lambda e, q=q: e.tensor_tensor_scan(
                        out=FTq[:], data0=FE[:], data1=FE[:],
                        initial=(0.0 if q == 0 else Flast[:]), op0=ALU.add, op1=ALU.min), [bf("FE"), bf("Flast")],
                        [bf("FT")])
                    sc.op("dve", lambda e: e.tensor_copy(out=Flast[:], in_=FTq[:, T - 1:T]), [bf("FT")], [bf("Flast")])
                    load(F_d[:, q * T:(q + 1) * T], bf("F_d"), FTq[:], bf("FT"))
                    for tb in range(NQB):
                        gb = q * NQB + tb
                        sc.op("pe", lambda e, gb=gb, tb=tb: e.matmul(PSG[:, 0:H], lhsT=FTq[0:H, tb * 128:(tb + 1) * 128],
                                                              rhs=IDf[0:H, 0:H], start=True, stop=True), [bf("FT"), bf("cst")],
                              [bf("psG")])
                        sc.op("dve", lambda e, gb=gb: e.tensor_copy(out=Ftok[:, gb, :], in_=PSG[:, 0:H]), [bf("psG")],
                              [bf("Ftok")])
                    sc.fence()
            for q in range(NQ):
                src, srcb = x_src(l, q)
                modpass(src, srcb, q, lambda k: lsc(l, 0, k), lambda k: lsc(l, 1, k), [bf("LS")])
                if sb_layer:
                    linear(lambda s: w_qkv[l, s], 3 * KC, KC, at_rhs, T, ep_qkv(q), bf("w_qkv"))
                    wo = w_ao[l]
                else:
                    linear(lambda s: w_bq[l - c.NA, s], KC, KC, at_rhs, T, ep_q, bf("w_bq"))
                    wo = w_bo[l - c.NA]
                attention(q, not sb_layer)
                linear(lambda s: wo[s], KC, KC, at_rhs, T,
                       lambda s, pa, pb: res_stats(s, pa, pb, lsc(l, 2, s), src, srcb, q), bf("w_o"))
                ln_finish()
                ln_apply(l, 0, q, xT_d, bf("xT_d%d" % q), True, moe)
                if moe:
                    gates()
                ffn(l, q, moe)
                pend = None
                for s in range(KC):
                    i = s % 2
                    load(SG[:] if i == 0 else HF[:], bf("SG") if i == 0 else bf("HF"), acc_d[s], bf("acc_d"))
                    p2 = res_stats(s, SG[:] if i == 0 else HF[:], bf("SG") if i == 0 else bf("HF"), lsc(l, 5, s), xT_d,
                                   bf("xT_d%d" % q), q)
                    if pend is not None:
                        pend()
                    pend = p2
                pend()
                sc.fence()
                ln_finish()
                if last:
                    ln_apply(l, 1, q, outT, bf("outT"), False, None)
                else:
                    ln_apply(l, 1, q, xT_d, bf("xT_d%d" % q), False, None)
        sc.fence()
        with nc.Block() as block:
            sc.emit(block)
    return nc


def _tile(W):
    K, N = W.shape
    return np.ascontiguousarray(W.reshape(K // 128, 128, N // 128, 128).transpose(2, 1, 0, 3))


def _colT(v):
    v = np.asarray(v, np.float32).reshape(-1, 128)
    return np.ascontiguousarray(v.T)


def prepare(cfg, inp):
    c = cfg
    D, KC, GC, NE = c.D, c.KC, c.GC, c.NE
    f = lambda a: np.asarray(a, np.float32)
    shared = {}
    shared["adabT"] = _colT(f(inp["ada_b"]))
    shared["tabT"] = _colT(f(inp["ada_table"]))
    shared["kvtabT"] = _colT(f(inp["kv_table"]))
    shared["lngT"] = _colT(f(inp["ln_g"]))
    shared["lnbT"] = _colT(f(inp["ln_b"]))
    shared["kvbf"] = np.ascontiguousarray(f(inp["kv_b_f"]).reshape(c.H, 1))
    rw = f(inp["moe_w_router"])
    shared["rw"] = np.ascontiguousarray(rw.reshape(rw.shape[0], KC, 128, NE).transpose(0, 2, 1, 3))
    rb = f(inp["moe_b_router"])
    shared["rb"] = np.ascontiguousarray(np.broadcast_to(rb[:, None, :], (rb.shape[0], 128, NE)))
    r = np.arange(128)
    cst = np.zeros((4, 128, 128), np.float32)
    cst[0] = np.eye(128)
    cst[1] = (r[None, :] < r[:, None])
    cst[2] = (r[None, :] <= r[:, None])
    cst[3] = 1.0
    shared["cst"] = cst
    shared["w_ada"] = _tile(f(inp["ada_w"]))
    shared["w_qkv"] = np.stack([_tile(w) for w in f(inp["a_w_qkv"])])
    shared["w_ao"] = np.stack([_tile(w) for w in f(inp["a_w_o"])])
    kvw = f(inp["kv_w"])
    shared["w_kv"] = _tile(kvw[:, :2 * D])
    shared["w_kvf"] = np.ascontiguousarray(kvw[:, 2 * D:].reshape(KC, 128, c.H).transpose(1, 0, 2))
    shared["w_bq"] = np.stack([_tile(w) for w in f(inp["b_w_q"])])
    shared["w_bo"] = np.stack([_tile(w) for w in f(inp["b_w_o"])])

    def up_tiles(W, ng, gofs, uofs):
        t = _tile(W)
        idx = []
        for g in range(ng):
            for j in range(GC):
                idx.append(gofs(g) // 128 + j)
                idx.append(uofs(g) // 128 + j)
        return t[np.asarray(idx)]

    def dn_tiles(W):
        return _tile(W)
    shared["w_fup"] = np.stack([up_tiles(w, c.NGD, lambda g: g * c.DFE, lambda g: 2 * D + g * c.DFE)
                                for w in f(inp["ffn_w_up"])])
    shared["w_fdn"] = np.stack([np.stack([dn_tiles(w[g * c.DFE:(g + 1) * c.DFE]) for g in range(c.NGD)])
                                for w in f(inp["ffn_w_down"])])
    shared["w_mup"] = np.stack([np.concatenate([up_tiles(w[e], 1, lambda g: 0, lambda g: c.DFE) for e in range(NE)])
                                for w in f(inp["moe_w_up"])])
    shared["w_mdn"] = np.stack([np.stack([dn_tiles(w[e]) for e in range(NE)]) for w in f(inp["moe_w_down"])])
    x = f(inp["x"])
    cc = f(inp["c"])
    maps = []
    for b in range(c.B):
        m = dict(shared)
        m["xT"] = np.ascontiguousarray(x[b].reshape(c.NQ, c.T, KC, 128).transpose(0, 2, 3, 1))
        m["cT"] = _colT(cc[b])
        maps.append(m)
    return maps


def run(cfg, inp):
    nc = build_nc(cfg)
    maps = prepare(cfg, inp)
    res = run_bass_kernel_spmd(nc, maps, core_ids=list(range(cfg.B)))
    outs = []
    for b in range(cfg.B):
        o = res.results[b]["outT"]
        outs.append(o.transpose(0, 3, 1, 2).reshape(cfg.S, cfg.D))
    return np.ascontiguousarray(np.stack(outs)).astype(np.float32)


def kernel(**inputs):
    return run(Cfg(), inputs)
```

```python
import numpy as np
from contextlib import ExitStack
import concourse.bass as bass
import concourse.mybir as mybir
from concourse.bass_utils import run_bass_kernel_spmd

F32 = mybir.dt.float32
BF16 = mybir.dt.bfloat16
AF = mybir.ActivationFunctionType
ALU = mybir.AluOpType
AX = mybir.AxisListType


class Cfg:
    def __init__(self, D=4096, S=4096, B=2, DEPTH=4, T=512):
        self.D, self.S, self.B, self.DEPTH = D, S, B, DEPTH
        self.H = D // 128
        self.KC = D // 128
        self.T = min(T, S)
        self.NQ = S // self.T
        self.NQB = self.T // 128
        self.NE = 8
        self.DFE = D // 2
        self.GC = self.DFE // 128
        self.NGD = (2 * D) // self.DFE
        self.NA = DEPTH // 2
        self.alpha = (2.0 * DEPTH) ** 0.25
        self.eps = 1e-5
        self.scale = 128 ** -0.5


class Buf:
    __slots__ = ("name", "w", "reads", "dsem", "dcnt")

    def __init__(self, name):
        self.name = name
        self.w = None
        self.reads = {}
        self.dsem = None
        self.dcnt = 0


class Rec:
    def __init__(self):
        self.call = None

    def __getattr__(self, name):
        def f(*a, **k):
            self.call = (name, a, k)
            return self
        return f


def _rec(fn):
    r = Rec()
    fn(r)
    assert r.call is not None
    return r.call


class Sched:
    ENG = ("pe", "act", "dve", "pool", "sp")

    def __init__(self, nc, stack):
        self.nc = nc
        self.stack = stack
        self.prog = {e: [] for e in self.ENG}
        self.esem = {e: stack.enter_context(nc.semaphore("es_" + e)) for e in ("pe", "act", "dve", "pool")}
        self.ecnt = {e: 0 for e in self.esem}
        self.waited = {e: {} for e in self.ENG}
        self.dsems = []
        self.semobj = {}
        self.bsem = None
        self.bcnt = 0

    def _sid(self, sem):
        k = id(sem)
        self.semobj[k] = sem
        return k

    def _need(self, reads, writes, skip_dma_waw=False):
        need = {}

        def add(tok):
            if tok is None:
                return
            k, v = tok
            if need.get(k, 0) < v:
                need[k] = v
        for b in reads:
            add(b.w)
        for b in writes:
            if not (skip_dma_waw and b.w is not None and b.dsem is not None and b.w[0] == id(b.dsem)):
                add(b.w)
            for k, v in b.reads.items():
                add((k, v))
        return need

    def _wait(self, e, need):
        for k, v in need.items():
            if e == "pe" and k == id(self.esem["pe"]):
                continue
            if self.waited[e].get(k, 0) >= v:
                continue
            self.waited[e][k] = v
            self.prog[e].append(("w", self.semobj[k], v))

    def op(self, e, fn, reads=(), writes=()):
        self._wait(e, self._need(reads, writes))
        self.ecnt[e] += 1
        sem = self.esem[e]
        tok = (self._sid(sem), self.ecnt[e])
        self.prog[e].append(("o", _rec(fn), sem, 1))
        for b in writes:
            b.w = tok
            b.reads = {}
        for b in reads:
            if b.reads.get(tok[0], 0) < tok[1]:
                b.reads[tok[0]] = tok[1]

    def dma(self, q, wbuf, rbuf, fn, waw=False):
        self._wait(q, self._need([rbuf], [wbuf], skip_dma_waw=not waw))
        if wbuf.dsem is None:
            wbuf.dsem = self.stack.enter_context(self.nc.semaphore("ds_%d" % len(self.dsems)))
            self.dsems.append(wbuf)
        wbuf.dcnt += 16
        tok = (self._sid(wbuf.dsem), wbuf.dcnt)
        self.prog[q].append(("o", _rec(fn), wbuf.dsem, 16))
        wbuf.w = tok
        wbuf.reads = {}
        if rbuf.reads.get(tok[0], 0) < tok[1]:
            rbuf.reads[tok[0]] = tok[1]

    def fence(self):
        need = {}
        for e, s in self.esem.items():
            if self.ecnt[e] > 0:
                need[self._sid(s)] = self.ecnt[e]
        for b in self.dsems:
            need[self._sid(b.dsem)] = b.dcnt
        self._wait("sp", need)
        if self.bsem is None:
            self.bsem = self.stack.enter_context(self.nc.semaphore("barrier"))
        self.bcnt += 1
        self.prog["sp"].append(("i", self.bsem, 1))
        for e in ("pe", "act", "dve", "pool"):
            self.prog[e].append(("w", self.bsem, self.bcnt))
            for k, v in need.items():
                if self.waited[e].get(k, 0) < v:
                    self.waited[e][k] = v

    def emit(self, block):
        nc = self.nc

        def replay(e, eng):
            for it in self.prog[e]:
                if it[0] == "w":
                    eng.wait_ge(it[1], it[2])
                elif it[0] == "i":
                    eng.sem_inc(it[1], it[2])
                else:
                    name, a, k = it[1]
                    getattr(eng, name)(*a, **k).then_inc(it[2], it[3])

        @block.tensor
        def _(eng):
            replay("pe", eng)

        @block.scalar
        def _(eng):
            replay("act", eng)

        @block.vector
        def _(eng):
            replay("dve", eng)

        @block.gpsimd
        def _(eng):
            replay("pool", eng)

        @block.sync
        def _(eng):
            replay("sp", eng)


def build_nc(cfg):
    c = cfg
    D, S, T, KC, H, NQ, NQB, GC, NE, DEPTH = c.D, c.S, c.T, c.KC, c.H, c.NQ, c.NQB, c.GC, c.NE, c.DEPTH
    NMOE = DEPTH // 2
    NDEN = (DEPTH + 1) // 2
    NB = DEPTH - c.NA
    nc = bass.Bass("TRN2", target_bir_lowering=False)

    def din(name, shape, dt=F32):
        return nc.dram_tensor(name, list(shape), dt, kind="ExternalInput").ap()

    def dint(name, shape, dt):
        return nc.dram_tensor(name, list(shape), dt, kind="Internal").ap()

    xT_in = din("xT", [NQ, KC, 128, T])
    cT_in = din("cT", [128, KC])
    adab_in = din("adabT", [128, 6 * KC])
    tab_in = din("tabT", [128, DEPTH * 6 * KC])
    kvtab_in = din("kvtabT", [128, 2 * KC])
    lng_in = din("lngT", [128, DEPTH * 2 * KC])
    lnb_in = din("lnbT", [128, DEPTH * 2 * KC])
    kvbf_in = din("kvbf", [H, 1])
    rw_in = din("rw", [NMOE, 128, KC, NE])
    rb_in = din("rb", [NMOE, 128, NE])
    cst_in = din("cst", [5, 128, 128])
    w_ada = din("w_ada", [6 * KC, 128, KC, 128])
    w_qkv = din("w_qkv", [c.NA, 3 * KC, 128, KC, 128])
    w_ao = din("w_ao", [c.NA, KC, 128, KC, 128])
    w_kv = din("w_kv", [2 * KC, 128, KC, 128])
    w_kvf = din("w_kvf", [128, KC, H])
    w_bq = din("w_bq", [NB, KC, 128, KC, 128])
    w_bo = din("w_bo", [NB, KC, 128, KC, 128])
    w_fup = din("w_fup", [NDEN, c.NGD * GC * 2, 128, KC, 128])
    w_fdn = din("w_fdn", [NDEN, c.NGD, KC, 128, GC, 128])
    w_mup = din("w_mup", [NMOE, NE * GC * 2, 128, KC, 128])
    w_mdn = din("w_mdn", [NMOE, NE, KC, 128, GC, 128])
    outT = nc.dram_tensor("outT", [NQ, KC, 128, T], F32, kind="ExternalOutput").ap()

    xT_d = dint("xT_d", [NQ, KC, 128, T], F32)
    yT_d = dint("yT_d", [KC, 128, T], F32)
    acc_d = dint("acc_d", [KC, 128, T], F32)
    qT_d = dint("qT_d", [H, 128, T], BF16)
    kT_d = dint("kT_d", [H, 128, S], BF16)
    v_d = dint("v_d", [H, S, 128], BF16)
    F_d = dint("F_d", [H, S], F32)
    wsrc = {"w_qkv": w_qkv, "w_ao": w_ao, "w_kv": w_kv, "w_bq": w_bq, "w_bo": w_bo, "w_fup": w_fup, "w_fdn": w_fdn,
            "w_mup": w_mup, "w_mdn": w_mdn}
    wpat = {4: "s p k n -> s p (k n)", 5: "a s p k n -> (a s) p (k n)", 6: "a g s p k n -> (a g s) p (k n)"}
    wflat = {n: a.rearrange(wpat[len(a.shape)]) for n, a in wsrc.items()}
    wbf = {}
    for n, a in wflat.items():
        ns_, nco = int(a.shape[0]), int(a.shape[2])
        per = max(1, (96 << 20) // (128 * nco * 2))
        wbf[n] = (per, [dint("%s_bf%d" % (n, k), [min(per, ns_ - k * per), 128, nco], BF16)
                        for k in range((ns_ + per - 1) // per)])

    def wtile(n, flat):
        per, lst = wbf[n]
        return lst[flat // per][flat % per]

    with ExitStack() as st:
        def sb(name, shape, dt):
            return st.enter_context(nc.sbuf_tensor("sb_" + name, list(shape), dt))

        def ps(name):
            return st.enter_context(nc.psum_tensor(name, [128, 512], F32))

        sc = Sched(nc, st)
        SMAX = max(S, KC * 128)
        AT = sb("AT", [128, KC, T], BF16)
        S0 = sb("S0", [128, SMAX], F32)
        S1 = sb("S1", [128, SMAX], F32)
        S2 = sb("S2", [128, max(SMAX, NE * T)], F32)
        R1 = sb("R1", [128, max(2 * S, GC * T)], BF16)
        R2 = sb("R2", [128, max(2 * S, 2 * KC * 128)], BF16)
        QQ = [sb("QQ%d" % i, [128, T], BF16) for i in range(2)]
        Wt = sb("Wt", [128, S], BF16)
        WB2 = sb("WB2", [128, KC * 128], BF16)
        WB3 = sb("WB3", [128, KC * 128], BF16)
        WT = [sb("WT%d" % i, [128, 512], BF16) for i in range(2)]
        FTq = sb("FTq", [H, T], F32)
        Flast = sb("Flast", [H, 1], F32)
        Ftok = sb("Ftok", [128, S // 128, H], F32)
        cst = sb("cst", [128, 5, 128], F32)
        cstb = sb("cstb", [128, 5, 128], BF16)
        small = {n: sb(n, [128, w], F32) for n, w in (
            ("cT", KC), ("adab", 6 * KC), ("tab", DEPTH * 6 * KC), ("kvtab", 2 * KC), ("lng", DEPTH * 2 * KC),
            ("lnb", DEPTH * 2 * KC), ("modT", 6 * KC), ("LS", DEPTH * 6 * KC), ("KVS", 2 * KC), ("AB", 2 * KC))}
        scb = sb("scb", [128, KC], BF16)
        kvbf = sb("kvbf", [H, 1], F32)
        RW = sb("RW", [128, KC, NE], F32)
        RB = sb("RB", [128, NE], F32)
        WFf = sb("WFf", [128, KC, H], F32)
        WFb = sb("WFb", [128, KC, H], BF16)
        XS = [sb("XS%d" % i, [128, T], F32) for i in range(2)]
        XA = sb("XA", [128, T], F32)
        YS = [sb("YS%d" % i, [128, T], F32) for i in range(2)]
        YB = sb("YB", [128, T], BF16)
        YQ = sb("YQ", [128, T], BF16)
        MEAN = sb("MEAN", [128, T], F32)
        RSTD = sb("RSTD", [128, T], F32)
        NMR = sb("NMR", [128, T], F32)
        TN = sb("TN", [128, T], F32)
        HF = sb("HF", [128, T], F32)
        SG = sb("SG", [128, T], F32)
        QS = [sb("QS%d" % i, [128, T], BF16) for i in range(2)]
        VTt = [sb("VTt%d" % i, [128, NQB, 128], BF16) for i in range(2)]
        FE = sb("FE", [H, T], F32)
        sm = {n: sb(n, [128, w], F32) for n, w in (("npt", 1), ("rs", 1), ("rinv", 1), ("lg", 8), ("m8", 8),
                                                    ("sel", 8), ("ex", 8), ("nm1", 1), ("G", 8))}
        DGf = sb("DGf", [128, 128], F32)
        DGb = sb("DGb", [128, 128], BF16)
        PSL = [ps("psL0"), ps("psL1")]
        PSS, PSQ, PSR, PSG, PSO = ps("psS"), ps("psQ"), ps("psR"), ps("psG"), ps("psO")
        PST = [PSS, PSQ]

        B = {}

        def bf(name):
            if name not in B:
                B[name] = Buf(name)
            return B[name]

        SPq, PLq = "sp", "pool"
        IDf, IDb = cst[:, 0, :], cstb[:, 0, :]
        MLTf, MLTb, MLEb = cst[:, 1, :], cstb[:, 1, :], cstb[:, 2, :]
        ONf, ONb = cst[:, 3, :], cstb[:, 3, :]
        MUTb = cstb[:, 4, :]

        def load(dst_ap, dst_b, src_ap, src_b, q=SPq):
            sc.dma(q, dst_b, src_b, lambda e, o=dst_ap, i=src_ap: e.dma_start(out=o, in_=i))

        load(cst[:], bf("cst"), cst_in.rearrange("c p n -> p c n"), bf("cst_in"))
        sc.op("dve", lambda e: e.tensor_copy(out=cstb[:], in_=cst[:]), [bf("cst")], [bf("cstb")])
        for n, src in (("cT", cT_in), ("adab", adab_in), ("tab", tab_in), ("kvtab", kvtab_in), ("lng", lng_in),
                       ("lnb", lnb_in)):
            load(small[n][:], bf(n), src, bf(n + "_in"))
        load(kvbf[:], bf("kvbf"), kvbf_in, bf("kvbf_in"))
        load(WFf[:], bf("WFf"), w_kvf, bf("w_kvf"))
        sc.op("dve", lambda e: e.tensor_copy(out=WFb[:], in_=WFf[:]), [bf("WFf")], [bf("WFb")])
        sc.op("dve", lambda e: e.tensor_scalar(out=kvbf[:], in0=kvbf[:], scalar1=-1.0, scalar2=None, op0=ALU.mult),
              [bf("kvbf")], [bf("kvbf")])
        sc.op("dve", lambda e: e.memset(S2[:, 0:1], 0.0), [], [bf("S2")])
        sc.op("act", lambda e: e.activation(out=scb[:], in_=small["cT"][:], func=AF.Silu), [bf("cT")], [bf("scb")])

        WFs = [S0, S1]
        WFb_ = [bf("S0"), bf("S1")]
        WBs = [R2[:, 0:KC * 128], R2[:, KC * 128:2 * KC * 128], WB2[:], WB3[:]]
        WBb_ = [bf("R2a"), bf("R2b"), bf("WB2"), bf("WB3")]
        PSLb = [bf("psL0"), bf("psL1")]

        def linear(tile_fn, nslabs, kcin, rhs_fn, N, epilogue, wbuf, f32w=False):
            pending = None
            NS = 2 if f32w else 4

            def ld(s):
                if f32w:
                    i = s % 2
                    load(WFs[i][:, 0:kcin * 128], WFb_[i], tile_fn(s).rearrange("p k n -> p (k n)"), wbuf)
                else:
                    i = s % NS
                    load(WBs[i][:, 0:kcin * 128], WBb_[i], tile_fn(s), wbuf)
            for s0 in range(min(NS - 1, nslabs)):
                ld(s0)
            for s in range(nslabs):
                i = s % NS
                if s + NS - 1 < nslabs:
                    ld(s + NS - 1)
                if f32w:
                    sc.op("pool", lambda e, i=i: e.tensor_copy(out=WBs[i][:, 0:kcin * 128], in_=WFs[i][:, 0:kcin * 128]),
                          [WFb_[i]], [WBb_[i]])
                if pending is not None:
                    pending()
                    pending = None
                pi = s % 2
                for kc in range(kcin):
                    rap, rb = rhs_fn(kc)
                    sc.op("pe", lambda e, i=i, kc=kc, rap=rap: e.matmul(
                        PSL[pi][:, 0:N], lhsT=WBs[i][:, kc * 128:(kc + 1) * 128], rhs=rap,
                        start=(kc == 0), stop=(kc == kcin - 1)), [WBb_[i], rb], [PSLb[pi]])
                pending = epilogue(s, PSL[pi][:, 0:N], PSLb[pi])
            if pending is not None:
                pending()
            sc.fence()

        for wn, src in wflat.items():
            ncols = int(src.shape[2])
            for s in range(int(src.shape[0])):
                i = s % 2
                load(WFs[i][:, 0:ncols], WFb_[i], src[s], bf(wn))
                sc.op("pool", lambda e, i=i: e.tensor_copy(out=WBs[i][:, 0:ncols], in_=WFs[i][:, 0:ncols]), [WFb_[i]],
                      [WBb_[i]])
                load(wtile(wn, s), bf(wn + "_bf"), WBs[i][:, 0:ncols], WBb_[i], q=PLq)
        sc.fence()

        def ep_mod(s, pa, pb):
            sc.op("dve", lambda e: e.tensor_tensor(out=small["modT"][:, s:s + 1], in0=pa, in1=small["adab"][:, s:s + 1],
                                                   op=ALU.add), [pb, bf("adab")], [bf("modT")])
            return None
        linear(lambda s: w_ada[s], 6 * KC, KC, lambda kc: (scb[:, kc:kc + 1], bf("scb")), 1, ep_mod, bf("w_ada"), f32w=True)
        LS = small["LS"]
        for l in range(DEPTH):
            o = l * 6 * KC
            sc.op("dve", lambda e, o=o: e.tensor_tensor(out=LS[:, o:o + 6 * KC], in0=small["modT"][:],
                                                        in1=small["tab"][:, o:o + 6 * KC], op=ALU.add),
                  [bf("modT"), bf("tab")], [bf("LS")])
            for i in (1, 2, 4, 5):
                sc.op("dve", lambda e, a=o + i * KC: e.tensor_scalar(out=LS[:, a:a + KC], in0=LS[:, a:a + KC], scalar1=1.0,
                                                                   scalar2=None, op0=ALU.add), [bf("LS")], [bf("LS")])
        KVS = small["KVS"]
        sc.op("dve", lambda e: e.tensor_tensor(out=KVS[:], in0=small["modT"][:, 0:2 * KC], in1=small["kvtab"][:],
                                               op=ALU.add), [bf("modT"), bf("kvtab")], [bf("KVS")])
        sc.op("dve", lambda e: e.tensor_scalar(out=KVS[:, KC:2 * KC], in0=KVS[:, KC:2 * KC], scalar1=1.0, scalar2=None,
                                               op0=ALU.add), [bf("KVS")], [bf("KVS")])
        sc.fence()

        def lsc(l, i, s):
            a = (l * 6 + i) * KC + s
            return LS[:, a:a + 1]

        ATb = [bf("AT%d" % k) for k in range(KC)]

        def x_src(l, q):
            return (xT_in, bf("xT_in")) if l == 0 else (xT_d, bf("xT_d%d" % q))

        def modpass(src, srcb, q, shift_fn, sc1_fn, sbufs):
            for k in range(KC):
                i = k % 2
                load(XS[i][:], bf("XS%d" % i), src[q, k], srcb)
                sc.op("act", lambda e, i=i, k=k: e.activation(out=AT[:, k, :], in_=XS[i][:], func=AF.Identity,
                                                              bias=shift_fn(k), scale=sc1_fn(k)),
                      [bf("XS%d" % i)] + sbufs, [ATb[k]])
            sc.fence()

        def at_rhs(kc):
            return AT[:, kc, :], ATb[kc]

        def store_fm(s_tile, s_buf, dst_ap, dst_b):
            def f():
                load(dst_ap, dst_b, s_tile, s_buf, q=PLq)
            return f

        def ep_q(s, pa, pb):
            i = s % 2
            sc.op("act", lambda e: e.activation(out=QS[i][:], in_=pa, func=AF.Copy), [pb], [bf("QS%d" % i)])
            return store_fm(QS[i][:], bf("QS%d" % i), qT_d[s % KC], bf("qT_d"))

        def ep_k(q):
            def f(s, pa, pb):
                i = s % 2
                sc.op("act", lambda e: e.activation(out=QS[i][:], in_=pa, func=AF.Copy), [pb], [bf("QS%d" % i)])
                return store_fm(QS[i][:], bf("QS%d" % i), kT_d[s % KC][:, q * T:(q + 1) * T], bf("kT_d"))
            return f

        def ep_v(q):
            def f(s, pa, pb):
                i = s % 2
                sc.op("act", lambda e: e.activation(out=QS[i][:], in_=pa, func=AF.Copy), [pb], [bf("QS%d" % i)])
                for tb in range(NQB):
                    sc.op("pe", lambda e, tb=tb: e.matmul(PSG[:, tb * 128:(tb + 1) * 128],
                                                          lhsT=QS[i][:, tb * 128:(tb + 1) * 128], rhs=IDb,
                                                          start=True, stop=True), [bf("QS%d" % i), bf("cstb")], [bf("psG")])
                sc.op("dve", lambda e: e.tensor_copy(out=VTt[i][:].rearrange("p a d -> p (a d)"), in_=PSG[:, 0:T]),
                      [bf("psG")], [bf("VTt%d" % i)])
                dst = v_d[s % KC][q * T:(q + 1) * T, :].rearrange("(a p) d -> p a d", p=128)
                return store_fm(VTt[i][:], bf("VTt%d" % i), dst, bf("v_d"))
            return f

        def ep_qkv(q):
            fk, fv = ep_k(q), ep_v(q)

            def f(s, pa, pb):
                return (ep_q, fk, fv)[s // KC](s, pa, pb)
            return f

        def ep_kv(q):
            fk, fv = ep_k(q), ep_v(q)

            def f(s, pa, pb):
                return (fk, fv)[s // KC](s, pa, pb)
            return f

        KTs = [R1[:, 0:S], R1[:, S:2 * S]]
        VVs = [R2[:, 0:S], R2[:, S:2 * S]]
        scale = c.scale

        def attention(q, fox):
            Lq = (q + 1) * T
            nkb_q = Lq // 128
            for h in range(H):
                i = h % 2
                KTb, VVb, QQb = bf("KT%d" % i), bf("VV%d" % i), bf("QQ%d" % i)
                load(KTs[i][:, 0:Lq], KTb, kT_d[h][:, 0:Lq], bf("kT_d"))
                load(VVs[i][:, 0:Lq].rearrange("p (a d) -> p a d", d=128), VVb,
                     v_d[h][0:Lq, :].rearrange("(a p) d -> p a d", p=128), bf("v_d"))
                load(QQ[i][:], QQb, qT_d[h], bf("qT_d"))
                if fox:
                    NFRi, NFRb = (S0, S1)[i], bf("S%d" % i)
                    load(NFRi[0:1, 0:T], NFRb, F_d[h:h + 1, q * T:(q + 1) * T], bf("F_d"))
                    sc.op("dve", lambda e: e.tensor_scalar(out=NFRi[0:1, 0:T], in0=NFRi[0:1, 0:T], scalar1=1.0 / scale,
                                                           scalar2=None, op0=ALU.mult), [NFRb], [NFRb])
                    for kb in range(nkb_q):
                        j = kb - q * NQB
                        t0 = max(j, 0) * 128
                        n = T - t0
                        pi = kb % 2
                        PTt, PTb = WT[pi], bf("WT%d" % pi)
                        sc.op("pe", lambda e: e.matmul(PSL[pi][:, 0:n], lhsT=KTs[i][:, kb * 128:(kb + 1) * 128],
                                                       rhs=QQ[i][:, t0:T], start=True, stop=False), [KTb, QQb], [PSLb[pi]])
                        sc.op("pe", lambda e: e.matmul(PSL[pi][:, 0:n], lhsT=ONf[0:1, :], rhs=NFRi[0:1, t0:T],
                                                       start=False, stop=True), [bf("cst"), NFRb], [PSLb[pi]])
                        sc.op("act", lambda e: e.activation(out=PTt[:, 0:n], in_=PSL[pi][:, 0:n], func=AF.Exp, scale=scale,
                                                            bias=Ftok[:, kb, h:h + 1]), [PSLb[pi], bf("Ftok")], [PTb])
                        if j >= 0:
                            sc.op("pool", lambda e: e.tensor_tensor(out=PTt[:, 0:128], in0=PTt[:, 0:128], in1=MUTb,
                                                                    op=ALU.mult), [PTb, bf("cstb")], [PTb])
                        sc.op("pe", lambda e: e.matmul(PSO[:, t0:T], lhsT=VVs[i][:, kb * 128:(kb + 1) * 128], rhs=PTt[:, 0:n],
                                                       start=(kb == 0), stop=(kb == nkb_q - 1)), [VVb, PTb], [bf("psO")])
                        sc.op("pe", lambda e: e.matmul(PSR[:, t0:T], lhsT=ONb, rhs=PTt[:, 0:n], start=(kb == 0),
                                                       stop=(kb == nkb_q - 1)), [bf("cstb"), PTb], [bf("psR")])
                    sc.op("dve", lambda e: e.reciprocal(out=TN[:], in_=PSR[:, 0:T]), [bf("psR")], [bf("TN")])
                    sc.op("dve", lambda e: e.tensor_tensor(out=AT[:, h, :], in0=PSO[:, 0:T], in1=TN[:], op=ALU.mult),
                          [bf("psO"), bf("TN")], [ATb[h]])
                    continue
                else:
                    sc.op("dve", lambda e: e.memset(S2[:, 0:1], 0.0), [], [bf("S2")])
                for qb in range(NQB):
                    g = q * NQB + qb
                    L = (g + 1) * 128
                    chunks = [(c0, min(512, L - c0)) for c0 in range(0, L, 512)]
                    qap = QQ[i][:, qb * 128:(qb + 1) * 128]

                    def zmm(ci, c0, w, with_f):
                        pi = ci % 2
                        sc.op("pe", lambda e: e.matmul(PSL[pi][:, 0:w], lhsT=qap, rhs=KTs[i][:, c0:c0 + w], start=True,
                                                       stop=not with_f), [QQb, KTb], [PSLb[pi]])
                        if with_f:
                            sc.op("pe", lambda e: e.matmul(PSL[pi][:, 0:w], lhsT=ONf[0:1, :], rhs=NFRi[0:1, c0:c0 + w],
                                                           start=False, stop=True), [bf("cst"), NFRb], [PSLb[pi]])
                        return pi
                    if not fox:
                        for ci, (c0, w) in enumerate(chunks):
                            pi = zmm(ci, c0, w, False)
                            sc.op("act", lambda e, pi=pi, c0=c0, w=w: e.activation(out=S0[:, c0:c0 + w], in_=PSL[pi][:, 0:w],
                                                                                func=AF.Exp, scale=scale),
                                  [PSLb[pi]], [bf("S0")])
                            sc.op("act", lambda e, c0=c0, w=w: e.activation(out=S1[:, c0:c0 + w], in_=S0[:, c0:c0 + w],
                                                                            func=AF.Ln, bias=1.0), [bf("S0")], [bf("S1")])
                        sc.op("pool", lambda e: e.tensor_tensor(out=S1[:, L - 128:L], in0=S1[:, L - 128:L], in1=MLTf,
                                                                op=ALU.mult), [bf("S1"), bf("cst")], [bf("S1")])
                        if L > 1:
                            sc.op("dve", lambda e: e.tensor_tensor_scan(out=S2[:, 1:L], data0=S1[:, 0:L - 1],
                                                                        data1=S1[:, 0:L - 1], initial=0.0, op0=ALU.add,
                                                                        op1=ALU.max), [bf("S1")], [bf("S2")])
                        sc.op("dve", lambda e: e.scalar_tensor_tensor(out=sm["npt"][:], in0=S2[:, L - 1:L], scalar=-1.0,
                                                                      in1=S1[:, L - 1:L], op0=ALU.mult, op1=ALU.subtract),
                              [bf("S1"), bf("S2")], [bf("npt")])
                        for ci, (c0, w) in enumerate(chunks):
                            pi = zmm(ci, c0, w, False)
                            sc.op("dve", lambda e, pi=pi, c0=c0, w=w: e.scalar_tensor_tensor(
                                out=S0[:, c0:c0 + w], in0=PSL[pi][:, 0:w], scalar=scale, in1=S2[:, c0:c0 + w],
                                op0=ALU.mult, op1=ALU.add), [PSLb[pi], bf("S2")], [bf("S0")])
                            sc.op("act", lambda e, c0=c0, w=w: e.activation(out=Wt[:, c0:c0 + w], in_=S0[:, c0:c0 + w],
                                                                            func=AF.Exp, bias=sm["npt"][:]),
                                  [bf("S0"), bf("npt")], [bf("Wt")])
                        sc.op("pool", lambda e: e.tensor_tensor(out=Wt[:, L - 128:L], in0=Wt[:, L - 128:L], in1=MLTb,
                                                                op=ALU.mult), [bf("Wt"), bf("cstb")], [bf("Wt")])
                        tr_rhs, tr_b = IDb, bf("cstb")
                    else:
                        for ci, (c0, w) in enumerate(chunks):
                            pi = zmm(ci, c0, w, True)
                            sc.op("act", lambda e, pi=pi, c0=c0, w=w: e.activation(
                                out=Wt[:, c0:c0 + w], in_=PSL[pi][:, 0:w], func=AF.Exp, scale=scale,
                                bias=Ftok[:, g, h:h + 1]), [PSLb[pi], bf("Ftok")], [bf("Wt")])
                        sc.op("pool", lambda e: e.tensor_tensor(out=Wt[:, L - 128:L], in0=Wt[:, L - 128:L], in1=MLEb,
                                                                op=ALU.mult), [bf("Wt"), bf("cstb")], [bf("Wt")])
                        sc.op("dve", lambda e: e.reduce_sum(out=sm["rs"][:], in_=Wt[:, 0:L], axis=AX.X), [bf("Wt")], [bf("rs")])
                        sc.op("dve", lambda e: e.reciprocal(out=sm["rinv"][:], in_=sm["rs"][:]), [bf("rs")], [bf("rinv")])
                        sc.op("dve", lambda e: e.tensor_scalar(out=DGb[:], in0=IDf, scalar1=sm["rinv"][:], scalar2=None,
                                                               op0=ALU.mult), [bf("cst"), bf("rinv")], [bf("DGb")])
                        tr_rhs, tr_b = DGb[:], bf("DGb")
                    for b0 in range(0, g + 1, 4):
                        nb = min(4, g + 1 - b0)
                        ti = (b0 // 4) % 2
                        tb_ = bf("psS") if ti == 0 else bf("psQ")
                        for j in range(nb):
                            kb = b0 + j
                            sc.op("pe", lambda e, j=j, kb=kb: e.matmul(PST[ti][:, j * 128:(j + 1) * 128],
                                                                       lhsT=Wt[:, kb * 128:(kb + 1) * 128], rhs=tr_rhs,
                                                                       start=True, stop=True), [bf("Wt"), tr_b], [tb_])
                        sc.op("act", lambda e, nb=nb: e.activation(out=WT[ti][:, 0:nb * 128], in_=PST[ti][:, 0:nb * 128],
                                                                   func=AF.Copy), [tb_], [bf("WT%d" % ti)])
                        for j in range(nb):
                            kb = b0 + j
                            sc.op("pe", lambda e, j=j, kb=kb: e.matmul(PSO[:, 0:128], lhsT=VVs[i][:, kb * 128:(kb + 1) * 128],
                                                                       rhs=WT[ti][:, j * 128:(j + 1) * 128], start=(kb == 0),
                                                                       stop=(kb == g)), [VVb, bf("WT%d" % ti)], [bf("psO")])
                    sc.op("dve", lambda e: e.tensor_copy(out=AT[:, h, qb * 128:(qb + 1) * 128], in_=PSO[:, 0:128]),
                          [bf("psO")], [ATb[h]])
            sc.fence()

        def res_stats(s, src_ap, src_b, gate_ap, xsrc, xsrcb, q):
            i = s % 2
            load(XS[i][:], bf("XS%d" % i), xsrc[q, s], xsrcb)
            sc.op("act", lambda e: e.activation(out=XA[:], in_=XS[i][:], func=AF.Identity, scale=c.alpha), [bf("XS%d" % i)],
                  [bf("XA")])
            sc.op("dve", lambda e: e.scalar_tensor_tensor(out=YS[i][:], in0=src_ap, scalar=gate_ap, in1=XA[:],
                                                          op0=ALU.mult, op1=ALU.add), [src_b, bf("XA"), bf("LS")],
                  [bf("YS%d" % i)])
            sc.op("act", lambda e: e.activation(out=YB[:], in_=YS[i][:], func=AF.Copy), [bf("YS%d" % i)], [bf("YB")])
            sc.op("act", lambda e: e.activation(out=YQ[:], in_=YS[i][:], func=AF.Square), [bf("YS%d" % i)], [bf("YQ")])
            sc.op("pe", lambda e: e.matmul(PSS[:, 0:T], lhsT=ONb, rhs=YB[:], start=(s == 0), stop=(s == KC - 1)),
                  [bf("cstb"), bf("YB")], [bf("psS")])
            sc.op("pe", lambda e: e.matmul(PSQ[:, 0:T], lhsT=ONb, rhs=YQ[:], start=(s == 0), stop=(s == KC - 1)),
                  [bf("cstb"), bf("YQ")], [bf("psQ")])
            return store_fm(YS[i][:], bf("YS%d" % i), yT_d[s], bf("yT_d"))

        def ln_finish():
            sc.op("dve", lambda e: e.tensor_scalar(out=MEAN[:], in0=PSS[:, 0:T], scalar1=1.0 / D, scalar2=None,
                                                   op0=ALU.mult), [bf("psS")], [bf("MEAN")])
            sc.op("dve", lambda e: e.tensor_tensor(out=TN[:], in0=MEAN[:], in1=MEAN[:], op=ALU.mult), [bf("MEAN")], [bf("TN")])
            sc.op("dve", lambda e: e.scalar_tensor_tensor(out=RSTD[:], in0=PSQ[:, 0:T], scalar=1.0 / D, in1=TN[:],
                                                          op0=ALU.mult, op1=ALU.subtract), [bf("psQ"), bf("TN")], [bf("RSTD")])
            sc.op("dve", lambda e: e.tensor_scalar(out=RSTD[:], in0=RSTD[:], scalar1=c.eps, scalar2=None, op0=ALU.add),
                  [bf("RSTD")], [bf("RSTD")])
            sc.op("act", lambda e: e.activation(out=RSTD[:], in_=RSTD[:], func=AF.Sqrt), [bf("RSTD")], [bf("RSTD")])
            sc.op("dve", lambda e: e.reciprocal(out=RSTD[:], in_=RSTD[:]), [bf("RSTD")], [bf("RSTD")])
            sc.op("dve", lambda e: e.scalar_tensor_tensor(out=NMR[:], in0=MEAN[:], scalar=-1.0, in1=RSTD[:], op0=ALU.mult,
                                                          op1=ALU.mult), [bf("MEAN"), bf("RSTD")], [bf("NMR")])
            sc.fence()

        def ln_apply(l, j, q, dst, dstb, make_h, router):
            AB = small["AB"]
            if make_h:
                go = (l * 2 + j) * KC
                sc.op("dve", lambda e: e.tensor_tensor(out=AB[:, 0:KC], in0=small["lng"][:, go:go + KC],
                                                       in1=LS[:, (l * 6 + 4) * KC:(l * 6 + 5) * KC], op=ALU.mult),
                      [bf("lng"), bf("LS")], [bf("AB")])
                sc.op("dve", lambda e: e.tensor_tensor(out=AB[:, KC:2 * KC], in0=small["lnb"][:, go:go + KC],
                                                       in1=LS[:, (l * 6 + 4) * KC:(l * 6 + 5) * KC], op=ALU.mult),
                      [bf("lnb"), bf("LS")], [bf("AB")])
                sc.op("dve", lambda e: e.tensor_tensor(out=AB[:, KC:2 * KC], in0=AB[:, KC:2 * KC],
                                                       in1=LS[:, (l * 6 + 3) * KC:(l * 6 + 4) * KC], op=ALU.add),
                      [bf("AB"), bf("LS")], [bf("AB")])
            pend = None
            for s in range(KC):
                i = s % 2
                go = (l * 2 + j) * KC + s
                load(YS[i][:], bf("YS%d" % i), yT_d[s], bf("yT_d"))
                sc.op("dve", lambda e: e.tensor_tensor(out=TN[:], in0=YS[i][:], in1=RSTD[:], op=ALU.mult),
                      [bf("YS%d" % i), bf("RSTD")], [bf("TN")])
                sc.op("dve", lambda e: e.tensor_tensor(out=TN[:], in0=TN[:], in1=NMR[:], op=ALU.add), [bf("TN"), bf("NMR")],
                      [bf("TN")])
                sc.op("dve", lambda e: e.tensor_scalar(out=XS[i][:], in0=TN[:], scalar1=small["lng"][:, go:go + 1],
                                                       scalar2=small["lnb"][:, go:go + 1], op0=ALU.mult, op1=ALU.add),
                      [bf("TN"), bf("lng"), bf("lnb")], [bf("XS%d" % i)])
                if pend is not None:
                    pend()
                pend = store_fm(XS[i][:], bf("XS%d" % i), dst[q, s], dstb)
                if make_h:
                    sc.op("dve", lambda e: e.tensor_scalar(out=HF[:], in0=TN[:], scalar1=AB[:, s:s + 1],
                                                           scalar2=AB[:, KC + s:KC + s + 1], op0=ALU.mult, op1=ALU.add),
                          [bf("TN"), bf("AB")], [bf("HF")])
                    sc.op("act", lambda e: e.activation(out=AT[:, s, :], in_=HF[:], func=AF.Copy), [bf("HF")], [ATb[s]])
                    if router:
                        for tb in range(NQB):
                            sc.op("pe", lambda e, tb=tb: e.matmul(
                                PSR[:, tb * NE:(tb + 1) * NE], lhsT=HF[:, tb * 128:(tb + 1) * 128], rhs=RW[:, s, :],
                                start=(s == 0 and tb == 0), stop=(s == KC - 1 and tb == NQB - 1), skip_group_check=True),
                                [bf("HF"), bf("RW")], [bf("psR")])
            pend()
            sc.fence()

        def gates():
            for tb in range(NQB):
                sc.op("dve", lambda e: e.tensor_tensor(out=sm["lg"][:], in0=PSR[:, tb * NE:(tb + 1) * NE], in1=RB[:],
                                                       op=ALU.add), [bf("psR"), bf("RB")], [bf("lg")])
                sc.op("dve", lambda e: e.max(out=sm["m8"][:], in_=sm["lg"][:]), [bf("lg")], [bf("m8")])
                sc.op("dve", lambda e: e.tensor_scalar(out=sm["sel"][:], in0=sm["lg"][:], scalar1=sm["m8"][:, 1:2],
                                                       scalar2=None, op0=ALU.is_ge), [bf("lg"), bf("m8")], [bf("sel")])
                sc.op("dve", lambda e: e.tensor_scalar(out=sm["nm1"][:], in0=sm["m8"][:, 0:1], scalar1=-1.0, scalar2=None,
                                                       op0=ALU.mult), [bf("m8")], [bf("nm1")])
                sc.op("act", lambda e: e.activation(out=sm["ex"][:], in_=sm["lg"][:], func=AF.Exp, bias=sm["nm1"][:]),
                      [bf("lg"), bf("nm1")], [bf("ex")])
                sc.op("dve", lambda e: e.tensor_tensor(out=sm["ex"][:], in0=sm["ex"][:], in1=sm["sel"][:], op=ALU.mult),
                      [bf("ex"), bf("sel")], [bf("ex")])
                sc.op("dve", lambda e: e.reduce_sum(out=sm["rs"][:], in_=sm["ex"][:], axis=AX.X), [bf("ex")], [bf("rs")])
                sc.op("dve", lambda e: e.reciprocal(out=sm["rinv"][:], in_=sm["rs"][:]), [bf("rs")], [bf("rinv")])
                sc.op("dve", lambda e: e.tensor_scalar(out=sm["G"][:], in0=sm["ex"][:], scalar1=sm["rinv"][:], scalar2=None,
                                                       op0=ALU.mult), [bf("ex"), bf("rinv")], [bf("G")])
                for ex in range(NE):
                    sc.op("dve", lambda e, ex=ex: e.tensor_scalar(out=DGf[:], in0=IDf, scalar1=sm["G"][:, ex:ex + 1],
                                                                  scalar2=None, op0=ALU.mult), [bf("cst"), bf("G")], [bf("DGf")])
                    sc.op("pe", lambda e: e.matmul(PSG[:, 0:128], lhsT=ONf, rhs=DGf[:], start=True, stop=True),
                          [bf("cst"), bf("DGf")], [bf("psG")])
                    sc.op("dve", lambda e, ex=ex: e.tensor_copy(out=S2[:, ex * T + tb * 128:ex * T + (tb + 1) * 128],
                                                                in_=PSG[:, 0:128]), [bf("psG")], [bf("S2")])
            sc.fence()

        ACTb = [bf("ACT%d" % k) for k in range(GC)]

        def ffn(l, q, moe):
            ng = NE if moe else c.NGD
            wi = l // 2
            upn, dnn = ("w_mup", "w_mdn") if moe else ("w_fup", "w_fdn")
            for g in range(ng):
                def ep_up(s, pa, pb):
                    jj = s // 2
                    if s % 2 == 0:
                        sc.op("act", lambda e: e.activation(out=SG[:], in_=pa, func=AF.Silu), [pb], [bf("SG")])
                    else:
                        sc.op("dve", lambda e: e.tensor_tensor(out=R1[:, jj * T:(jj + 1) * T], in0=SG[:], in1=pa, op=ALU.mult),
                              [bf("SG"), pb], [ACTb[jj]])
                    return None
                linear(lambda s, g=g: wtile(upn, (wi * ng + g) * GC * 2 + s), GC * 2, KC, at_rhs, T, ep_up, bf("w_up"))

                def ep_dn(s, pa, pb, g=g):
                    i = s % 2
                    if moe:
                        sc.op("dve", lambda e: e.tensor_tensor(out=YS[i][:], in0=pa, in1=S2[:, g * T:(g + 1) * T], op=ALU.mult),
                              [pb, bf("S2")], [bf("YS%d" % i)])
                    else:
                        sc.op("dve", lambda e: e.tensor_copy(out=YS[i][:], in_=pa), [pb], [bf("YS%d" % i)])

                    def f():
                        if g == 0:
                            load(acc_d[s], bf("acc_d"), YS[i][:], bf("YS%d" % i), q=PLq)
                        else:
                            sc.dma(PLq, bf("acc_d"), bf("YS%d" % i),
                                   lambda e: e.dma_start(out=acc_d[s], in_=YS[i][:], accum_op=ALU.add))
                    return f
                linear(lambda s, g=g: wtile(dnn, (wi * ng + g) * KC + s), KC, GC, lambda kc: (R1[:, kc * T:(kc + 1) * T], ACTb[kc]), T, ep_dn,
                       bf("w_dn"))

        ri = 0
        for l in range(DEPTH):
            sb_layer = l < c.NA
            moe = (l % 2 == 1)
            last = (l == DEPTH - 1)
            if moe:
                load(RW[:], bf("RW"), rw_in[l // 2], bf("rw_in"))
                load(RB[:], bf("RB"), rb_in[l // 2], bf("rb_in"))
            if l == c.NA:
                for q in range(NQ):
                    src, srcb = x_src(l, q)
                    modpass(src, srcb, q, lambda k: KVS[:, k:k + 1], lambda k: KVS[:, KC + k:KC + k + 1], [bf("KVS")])
                    linear(lambda s: wtile("w_kv", s), 2 * KC, KC, at_rhs, T, ep_kv(q), bf("w_kv_bf"))
                    for kc in range(KC):
                        sc.op("pe", lambda e, kc=kc: e.matmul(PSG[0:H, 0:T], lhsT=WFb[:, kc, :], rhs=AT[:, kc, :],
                                                              start=(kc == 0), stop=(kc == KC - 1)), [bf("WFb"), ATb[kc]],
                              [bf("psG")])
                    sc.op("act", lambda e: e.activation(out=FE[:], in_=PSG[0:H, 0:T], func=AF.Exp, scale=-1.0, bias=kvbf[:]),
                          [bf("psG"), bf("kvbf")], [bf("FE")])
                    sc.op("act", lambda e: e.activation(out=FE[:], in_=FE[:], func=AF.Ln, bias=1.0), [bf("FE")], [bf("FE")])
                    sc.op("dve", lambda e: e.tensor_scalar(out=FE[:], in0=FE[:], scalar1=-1.0, scalar2=None, op0=ALU.mult),
                          [bf("FE")], [bf("FE")])
                    sc.op("dve", lambda e, q=q: e.tensor_tensor_scan(
                        out=FTq[:], data0=FE[:], data1=FE[:],
                        initial=(0.0 if q == 0 else Flast[:]), op0=ALU.add, op1=ALU.min), [bf("FE"), bf("Flast")],
                        [bf("FT")])
                    sc.op("dve", lambda e: e.tensor_copy(out=Flast[:], in_=FTq[:, T - 1:T]), [bf("FT")], [bf("Flast")])
                    load(F_d[:, q * T:(q + 1) * T], bf("F_d"), FTq[:], bf("FT"))
                    for tb in range(NQB):
                        gb = q * NQB + tb
                        sc.op("pe", lambda e, gb=gb, tb=tb: e.matmul(PSG[:, 0:H], lhsT=FTq[0:H, tb * 128:(tb + 1) * 128],
                                                              rhs=IDf[0:H, 0:H], start=True, stop=True), [bf("FT"), bf("cst")],
                              [bf("psG")])
                        sc.op("dve", lambda e, gb=gb: e.tensor_scalar(out=Ftok[:, gb, :], in0=PSG[:, 0:H], scalar1=-1.0, scalar2=None,
                                                                    op0=ALU.mult), [bf("psG")],
                              [bf("Ftok")])
                    sc.fence()
            for q in range(NQ):
                src, srcb = x_src(l, q)
                modpass(src, srcb, q, lambda k: lsc(l, 0, k), lambda k: lsc(l, 1, k), [bf("LS")])
                if sb_layer:
                    linear(lambda s: wtile("w_qkv", l * 3 * KC + s), 3 * KC, KC, at_rhs, T, ep_qkv(q), bf("w_qkv_bf"))
                    won, wol = "w_ao", l
                else:
                    linear(lambda s: wtile("w_bq", (l - c.NA) * KC + s), KC, KC, at_rhs, T, ep_q, bf("w_bq_bf"))
                    won, wol = "w_bo", l - c.NA
                attention(q, not sb_layer)
                linear(lambda s: wtile(won, wol * KC + s), KC, KC, at_rhs, T,
                       lambda s, pa, pb: res_stats(s, pa, pb, lsc(l, 2, s), src, srcb, q), bf("w_o"))
                ln_finish()
                ln_apply(l, 0, q, xT_d, bf("xT_d%d" % q), True, moe)
                if moe:
                    gates()
                ffn(l, q, moe)
                pend = None
                for s in range(KC):
                    i = s % 2
                    load(SG[:] if i == 0 else HF[:], bf("SG") if i == 0 else bf("HF"), acc_d[s], bf("acc_d"))
                    p2 = res_stats(s, SG[:] if i == 0 else HF[:], bf("SG") if i == 0 else bf("HF"), lsc(l, 5, s), xT_d,
                                   bf("xT_d%d" % q), q)
                    if pend is not None:
                        pend()
                    pend = p2
                pend()
                sc.fence()
                ln_finish()
                if last:
                    ln_apply(l, 1, q, outT, bf("outT"), False, None)
                else:
                    ln_apply(l, 1, q, xT_d, bf("xT_d%d" % q), False, None)
        sc.fence()
        with nc.Block() as block:
            sc.emit(block)
    return nc


def _tile(W):
    K, N = W.shape
    return np.ascontiguousarray(W.reshape(K // 128, 128, N // 128, 128).transpose(2, 1, 0, 3))


def _colT(v):
    v = np.asarray(v, np.float32).reshape(-1, 128)
    return np.ascontiguousarray(v.T)


def prepare(cfg, inp):
    c = cfg
    D, KC, GC, NE = c.D, c.KC, c.GC, c.NE
    f = lambda a: np.asarray(a, np.float32)
    shared = {}
    shared["adabT"] = _colT(f(inp["ada_b"]))
    shared["tabT"] = _colT(f(inp["ada_table"]))
    shared["kvtabT"] = _colT(f(inp["kv_table"]))
    shared["lngT"] = _colT(f(inp["ln_g"]))
    shared["lnbT"] = _colT(f(inp["ln_b"]))
    shared["kvbf"] = np.ascontiguousarray(f(inp["kv_b_f"]).reshape(c.H, 1))
    rw = f(inp["moe_w_router"])
    shared["rw"] = np.ascontiguousarray(rw.reshape(rw.shape[0], KC, 128, NE).transpose(0, 2, 1, 3))
    rb = f(inp["moe_b_router"])
    shared["rb"] = np.ascontiguousarray(np.broadcast_to(rb[:, None, :], (rb.shape[0], 128, NE)))
    r = np.arange(128)
    cst = np.zeros((5, 128, 128), np.float32)
    cst[0] = np.eye(128)
    cst[1] = (r[None, :] < r[:, None])
    cst[2] = (r[None, :] <= r[:, None])
    cst[3] = 1.0
    cst[4] = (r[:, None] <= r[None, :])
    shared["cst"] = cst
    shared["w_ada"] = _tile(f(inp["ada_w"]))
    shared["w_qkv"] = np.stack([_tile(w) for w in f(inp["a_w_qkv"])])
    shared["w_ao"] = np.stack([_tile(w) for w in f(inp["a_w_o"])])
    kvw = f(inp["kv_w"])
    shared["w_kv"] = _tile(kvw[:, :2 * D])
    shared["w_kvf"] = np.ascontiguousarray(kvw[:, 2 * D:].reshape(KC, 128, c.H).transpose(1, 0, 2))
    shared["w_bq"] = np.stack([_tile(w) for w in f(inp["b_w_q"])])
    shared["w_bo"] = np.stack([_tile(w) for w in f(inp["b_w_o"])])

    def up_tiles(W, ng, gofs, uofs):
        t = _tile(W)
        idx = []
        for g in range(ng):
            for j in range(GC):
                idx.append(gofs(g) // 128 + j)
                idx.append(uofs(g) // 128 + j)
        return t[np.asarray(idx)]

    def dn_tiles(W):
        return _tile(W)
    shared["w_fup"] = np.stack([up_tiles(w, c.NGD, lambda g: g * c.DFE, lambda g: 2 * D + g * c.DFE)
                                for w in f(inp["ffn_w_up"])])
    shared["w_fdn"] = np.stack([np.stack([dn_tiles(w[g * c.DFE:(g + 1) * c.DFE]) for g in range(c.NGD)])
                                for w in f(inp["ffn_w_down"])])
    shared["w_mup"] = np.stack([np.concatenate([up_tiles(w[e], 1, lambda g: 0, lambda g: c.DFE) for e in range(NE)])
                                for w in f(inp["moe_w_up"])])
    shared["w_mdn"] = np.stack([np.stack([dn_tiles(w[e]) for e in range(NE)]) for w in f(inp["moe_w_down"])])
    x = f(inp["x"])
    cc = f(inp["c"])
    maps = []
    for b in range(c.B):
        m = dict(shared)
        m["xT"] = np.ascontiguousarray(x[b].reshape(c.NQ, c.T, KC, 128).transpose(0, 2, 3, 1))
        m["cT"] = _colT(cc[b])
        maps.append(m)
    return maps


def run(cfg, inp):
    nc = build_nc(cfg)
    maps = prepare(cfg, inp)
    res = run_bass_kernel_spmd(nc, maps, core_ids=list(range(cfg.B)))
    outs = []
    for b in range(cfg.B):
        o = res.results[b]["outT"]
        outs.append(o.transpose(0, 3, 1, 2).reshape(cfg.S, cfg.D))
    return np.ascontiguousarray(np.stack(outs)).astype(np.float32)


def kernel(**inputs):
    return run(Cfg(), inputs)
```
